# Optimizing a Trainium2 kernel written in Bass

```python
import jax, jax.numpy as jnp
from jax import lax
import numpy as np

D_MODEL = 4096
BATCH = 4
SEQ = 4096
DEPTH = 4

CTX_LEN = 256
GRID_W = 64
HEAD_DIM = 128
MIX_WIDTH = D_MODEL

SWA_HEADS = MIX_WIDTH // 2 // HEAD_DIM
SWA_KV_HEADS = SWA_HEADS // 4
SWA_Q = SWA_HEADS * HEAD_DIM
SWA_KV = SWA_KV_HEADS * HEAD_DIM
SWA_WINDOW = 128
SWA_BLOCK = 128

CONV_W = MIX_WIDTH // 4
CONV_K = 3

NAT_HEADS = MIX_WIDTH // 4 // HEAD_DIM
NAT_W = NAT_HEADS * HEAD_DIM
NAT_ROWS_MAX = 8
NAT_COLS = 16
NAT_COL_BLOCK = 16
NAT_KEY_COLS = 32

OFF_SWA_K = SWA_Q
OFF_SWA_V = OFF_SWA_K + SWA_KV
OFF_CONV = OFF_SWA_V + SWA_KV
OFF_NAT_Q = OFF_CONV + 3 * CONV_W
OFF_NAT_K = OFF_NAT_Q + NAT_W
OFF_NAT_V = OFF_NAT_K + NAT_W
IN_COLS = OFF_NAT_V + NAT_W

ROPE_THETA = 10000.0
ROPE_FREQS = HEAD_DIM // 4

N_GROUPS = 4
EXPERTS_PER_GROUP = 4
N_EXPERTS = N_GROUPS * EXPERTS_PER_GROUP
TOP_K_EXPERT = 2
D_EXPERT = D_MODEL // 16

DEEPNORM_ALPHA = (2.0 * DEPTH) ** 0.25
DEEPNORM_BETA = (8.0 * DEPTH) ** -0.25
LN_EPS = 1e-5
RMS_EPS = 1e-6
ADA_INIT = 0.5
NEG_INF = -1e30

kernel_name = "hybrid_swa_conv_nat_hmoe_dit"


def layer_norm(x, g, b):
    xf = x.astype(jnp.float32)
    mu = jnp.mean(xf, axis=-1, keepdims=True)
    var = jnp.mean(jnp.square(xf - mu), axis=-1, keepdims=True)
    y = (xf - mu) * lax.rsqrt(var + LN_EPS)
    return (y * g.astype(jnp.float32) + b.astype(jnp.float32)).astype(x.dtype)


def rms_norm(x, g):
    xf = x.astype(jnp.float32)
    y = xf * lax.rsqrt(jnp.mean(jnp.square(xf), axis=-1, keepdims=True) + RMS_EPS)
    return (y * g.astype(jnp.float32)).astype(x.dtype)


def modulate(h, shift, scale):
    return h * (1 + scale) + shift


def split_heads(t, n_heads):
    return t.reshape(t.shape[:-1] + (n_heads, HEAD_DIM))


def joint_softmax(*logits):
    z = jnp.concatenate([l.astype(jnp.float32) for l in logits], axis=-1)
    return jax.nn.softmax(z, axis=-1)


def axial_rope_tables(n_tokens):
    t = jnp.arange(n_tokens, dtype=jnp.int32)
    pos = jnp.stack([t // GRID_W, t % GRID_W], axis=-1).astype(jnp.float32)
    inv_freq = ROPE_THETA ** (-jnp.arange(ROPE_FREQS, dtype=jnp.float32) / ROPE_FREQS)
    ang = pos[:, :, None] * inv_freq
    return jnp.cos(ang), jnp.sin(ang)


def apply_axial_rope(x, cos, sin):
    xs = x.reshape(x.shape[:-1] + (2, 2, ROPE_FREQS))
    x1, x2 = xs[..., 0, :], xs[..., 1, :]
    cs = cos[:, None].astype(x.dtype)
    sn = sin[:, None].astype(x.dtype)
    out = jnp.stack([x1 * cs - x2 * sn, x2 * cs + x1 * sn], axis=-2)
    return out.reshape(x.shape)


def context_attention(q, k, v, sink):
    Bn, L, HQ, _ = q.shape
    HKV = k.shape[2]
    G = HQ // HKV
    qg = (q * HEAD_DIM ** -0.5).reshape(Bn, L, HKV, G, HEAD_DIM)
    s = jnp.einsum('bqhgd,bkhd->bhgqk', qg, k)
    parts = [s]
    if sink is not None:
        parts.append(jnp.broadcast_to(sink.reshape(HKV, G, 1, 1), s.shape[:-1] + (1,)))
    p = joint_softmax(*parts)[..., :L].astype(v.dtype)
    o = jnp.einsum('bhgqk,bkhd->bqhgd', p, v)
    return o.reshape(Bn, L, HQ * HEAD_DIM)


def window_attention(q, k, v, kc, vc, sink):
    Bn, S, HQ, _ = q.shape
    HKV = k.shape[2]
    G = HQ // HKV
    nb = S // SWA_BLOCK
    qb = (q * HEAD_DIM ** -0.5).reshape(Bn, nb, SWA_BLOCK, HKV, G, HEAD_DIM)
    pad = ((0, 0), (SWA_BLOCK, SWA_BLOCK), (0, 0), (0, 0))
    kp = jnp.pad(k, pad).reshape(Bn, nb + 2, SWA_BLOCK, HKV, HEAD_DIM)
    vp = jnp.pad(v, pad).reshape(Bn, nb + 2, SWA_BLOCK, HKV, HEAD_DIM)
    kband = jnp.concatenate([kp[:, :-2], kp[:, 1:-1], kp[:, 2:]], axis=2)
    vband = jnp.concatenate([vp[:, :-2], vp[:, 1:-1], vp[:, 2:]], axis=2)
    s_win = jnp.einsum('bnqhgd,bnkhd->bnhgqk', qb, kband).astype(jnp.float32)
    qi = jnp.arange(SWA_BLOCK)[:, None]
    kj = jnp.arange(3 * SWA_BLOCK)[None, :]
    in_band = jnp.abs(kj - SWA_BLOCK - qi) <= SWA_WINDOW
    kpos = (jnp.arange(nb)[:, None, None] - 1) * SWA_BLOCK + kj[None]
    valid = in_band[None] & (kpos >= 0) & (kpos < S)
    s_win = jnp.where(valid[None, :, None, None], s_win, NEG_INF)
    s_ctx = jnp.einsum('bnqhgd,bkhd->bnhgqk', qb, kc)
    s_sink = jnp.broadcast_to(sink.reshape(1, 1, HKV, G, 1, 1), s_ctx.shape[:-1] + (1,))
    p = joint_softmax(s_win, s_ctx, s_sink).astype(v.dtype)
    nk = 3 * SWA_BLOCK
    L = kc.shape[1]
    o = (jnp.einsum('bnhgqk,bnkhd->bnqhgd', p[..., :nk], vband)
         + jnp.einsum('bnhgqk,bkhd->bnqhgd', p[..., nk:nk + L], vc))
    return o.reshape(Bn, S, HQ * HEAD_DIM)


def neighbourhood_attention(q, k, v, kc, vc, rpb):
    Bn, S, H, _ = q.shape
    rows = S // GRID_W
    wr = min(NAT_ROWS_MAX, rows)
    ncb = GRID_W // NAT_COL_BLOCK
    nk = wr * NAT_KEY_COLS
    kg = k.reshape(Bn, rows, GRID_W, H, HEAD_DIM)
    vg = v.reshape(Bn, rows, GRID_W, H, HEAD_DIM)
    qg = jnp.moveaxis((q * HEAD_DIM ** -0.5).reshape(Bn, rows, ncb, NAT_COL_BLOCK, H, HEAD_DIM), 1, 0)
    qcol = jnp.arange(GRID_W).reshape(ncb, NAT_COL_BLOCK)
    col_start = jnp.clip(qcol - NAT_COLS // 2, 0, GRID_W - NAT_COLS)
    strip_start = jnp.clip(jnp.arange(ncb) * NAT_COL_BLOCK - NAT_COLS // 2, 0, GRID_W - NAT_KEY_COLS)
    kcol = strip_start[:, None] + jnp.arange(NAT_KEY_COLS)[None, :]
    col_ok = ((kcol[:, None, :] >= col_start[..., None])
              & (kcol[:, None, :] < col_start[..., None] + NAT_COLS))
    mask = jnp.broadcast_to(col_ok[:, :, None, :], (ncb, NAT_COL_BLOCK, wr, NAT_KEY_COLS)).reshape(ncb, NAT_COL_BLOCK, nk)
    dc_idx = jnp.clip(kcol[:, None, :] - qcol[..., None] + NAT_COLS - 1, 0, 2 * NAT_COLS - 2)

    def row_block(args):
        r, q_r = args
        rs = jnp.clip(r - wr // 2, 0, rows - wr)
        k_rows = lax.dynamic_slice_in_dim(kg, rs, wr, axis=1)
        v_rows = lax.dynamic_slice_in_dim(vg, rs, wr, axis=1)
        k_blk = jnp.transpose(k_rows[:, :, kcol], (0, 2, 1, 3, 4, 5)).reshape(Bn, ncb, nk, H, HEAD_DIM)
        v_blk = jnp.transpose(v_rows[:, :, kcol], (0, 2, 1, 3, 4, 5)).reshape(Bn, ncb, nk, H, HEAD_DIM)
        dr_idx = rs + jnp.arange(wr) - r + NAT_ROWS_MAX - 1
        bias = rpb[:, dr_idx[:, None, None, None], dc_idx[None]]
        bias = jnp.transpose(bias, (2, 0, 3, 1, 4)).reshape(ncb, H, NAT_COL_BLOCK, nk)
        s = jnp.einsum('bnqhd,bnkhd->bnhqk', q_r, k_blk).astype(jnp.float32) + bias[None].astype(jnp.float32)
        s = jnp.where(mask[None, :, None], s, NEG_INF)
        s_ctx = jnp.einsum('bnqhd,bkhd->bnhqk', q_r, kc)
        p = joint_softmax(s, s_ctx).astype(v.dtype)
        return (jnp.einsum('bnhqk,bnkhd->bnqhd', p[..., :nk], v_blk)
                + jnp.einsum('bnhqk,bkhd->bnqhd', p[..., nk:], vc))

    out = lax.map(row_block, (jnp.arange(rows, dtype=jnp.int32), qg))
    return jnp.moveaxis(out, 0, 1).reshape(Bn, S, H * HEAD_DIM)


def short_conv(z, w, b):
    T = z.shape[1]
    pad = CONV_K // 2
    zp = jnp.pad(z, ((0, 0), (pad, pad), (0, 0)))
    out = b
    for j in range(CONV_K):
        out = out + zp[:, j:j + T] * w[j]
    return out


def gated_short_conv(u3, w, b):
    u, b_gate, c_gate = jnp.split(u3, 3, axis=-1)
    return b_gate * short_conv(c_gate * u, w, b)


def mix_output(y_swa, y_conv, y_nat, g, w_out):
    y = jnp.concatenate([
        rms_norm(y_swa, g[:SWA_Q]),
        rms_norm(y_conv, g[SWA_Q:SWA_Q + CONV_W]),
        rms_norm(y_nat, g[SWA_Q + CONV_W:]),
    ], axis=-1)
    return y @ w_out


def hierarchical_moe(h, w_rg, b_rg, w_re, b_re, w_gate, w_up, w_down):
    lead = h.shape[:-1]
    D = h.shape[-1]
    t = h.reshape(-1, D)
    N = t.shape[0]
    g_prob = jax.nn.softmax((t @ w_rg).astype(jnp.float32) + b_rg.astype(jnp.float32), axis=-1)
    g_p, g_idx = lax.top_k(g_prob, 1)
    onehot_g = jax.nn.one_hot(g_idx[:, 0], N_GROUPS, dtype=jnp.float32)
    e_logits = ((t @ w_re).astype(jnp.float32) + b_re.astype(jnp.float32)).reshape(N, N_GROUPS, EXPERTS_PER_GROUP)
    e_in = jnp.einsum('ng,nge->ne', onehot_g, e_logits)
    e_val, e_idx = lax.top_k(e_in, TOP_K_EXPERT)
    e_w = jax.nn.softmax(e_val, axis=-1) * g_p
    within = jnp.sum(jax.nn.one_hot(e_idx, EXPERTS_PER_GROUP, dtype=jnp.float32) * e_w[..., None], axis=1)
    gates = (onehot_g[:, :, None] * within[:, None, :]).reshape(N, N_EXPERTS).astype(t.dtype)
    hid = jax.nn.silu(jnp.einsum('nd,edf->nef', t, w_gate)) * jnp.einsum('nd,edf->nef', t, w_up)
    out = jnp.einsum('nef,efd->nd', hid * gates[..., None], w_down)
    return out.reshape(lead + (D,))


def setup_inputs(seed: int = 0) -> dict:
    key = jax.random.key(seed)
    ks = jax.random.split(key, 24)
    f32 = jnp.float32
    nrm = jax.random.normal
    D = D_MODEL
    return {
        "x": nrm(ks[0], (BATCH, SEQ, D), f32),
        "c": nrm(ks[1], (BATCH, D), f32),
        "ctx": nrm(ks[2], (BATCH, CTX_LEN, D), f32),
        "c_ctx": nrm(ks[3], (D,), f32),
        "w_ada": nrm(ks[4], (DEPTH, D, 6 * D), f32) * (ADA_INIT * D ** -0.5),
        "b_ada": nrm(ks[5], (DEPTH, 6 * D), f32) * 0.01,
        "w_in": nrm(ks[6], (DEPTH, D, IN_COLS), f32) * D ** -0.5,
        "conv_w": nrm(ks[7], (DEPTH, CONV_K, CONV_W), f32) * CONV_K ** -0.5,
        "conv_b": nrm(ks[8], (DEPTH, CONV_W), f32) * 0.01,
        "attn_sink": nrm(ks[9], (DEPTH, SWA_HEADS), f32) * 0.5,
        "nat_rpb": nrm(ks[10], (DEPTH, NAT_HEADS, 2 * NAT_ROWS_MAX - 1, 2 * NAT_COLS - 1), f32) * 0.1,
        "mix_norm_g": 1.0 + 0.02 * nrm(ks[11], (DEPTH, MIX_WIDTH), f32),
        "w_out": nrm(ks[12], (DEPTH, MIX_WIDTH, D), f32) * (MIX_WIDTH ** -0.5 * DEEPNORM_BETA),
        "ln1_g": 1.0 + 0.02 * nrm(ks[13], (DEPTH, D), f32),
        "ln1_b": 0.02 * nrm(ks[14], (DEPTH, D), f32),
        "w_router_group": nrm(ks[15], (DEPTH, D, N_GROUPS), f32) * D ** -0.5,
        "b_router_group": nrm(ks[16], (DEPTH, N_GROUPS), f32) * 0.01,
        "w_router_expert": nrm(ks[17], (DEPTH, D, N_EXPERTS), f32) * D ** -0.5,
        "b_router_expert": nrm(ks[18], (DEPTH, N_EXPERTS), f32) * 0.01,
        "w_gate": nrm(ks[19], (DEPTH, N_EXPERTS, D, D_EXPERT), f32) * D ** -0.5,
        "w_up": nrm(ks[20], (DEPTH, N_EXPERTS, D, D_EXPERT), f32) * D ** -0.5,
        "w_down": nrm(ks[21], (DEPTH, N_EXPERTS, D_EXPERT, D), f32) * (D_EXPERT ** -0.5 * DEEPNORM_BETA),
        "ln2_g": 1.0 + 0.02 * nrm(ks[22], (DEPTH, D), f32),
        "ln2_b": 0.02 * nrm(ks[23], (DEPTH, D), f32),
    }


def reference(x, c, ctx, c_ctx, w_ada, b_ada, w_in, conv_w, conv_b, attn_sink, nat_rpb, mix_norm_g, w_out,
              ln1_g, ln1_b, w_router_group, b_router_group, w_router_expert, b_router_expert,
              w_gate, w_up, w_down, ln2_g, ln2_b):
    D = x.shape[-1]
    cos, sin = axial_rope_tables(x.shape[1])
    act_lat = jax.nn.silu(c)
    act_ctx = jax.nn.silu(c_ctx)
    h, hc = x, ctx
    for i in range(DEPTH):
        last = i == DEPTH - 1
        w_in_i = w_in[i]
        moe_params = (w_router_group[i], b_router_group[i], w_router_expert[i], b_router_expert[i],
                      w_gate[i], w_up[i], w_down[i])
        mod = act_lat @ w_ada[i] + b_ada[i]
        sh_m, sc_m, g_m, sh_f, sc_f, g_f = jnp.split(mod[:, None, :], 6, axis=-1)
        n_mod_c = 2 * D if last else 6 * D
        mod_c = (act_ctx @ w_ada[i][:, :n_mod_c] + b_ada[i][:n_mod_c]).reshape(-1, D)
        ctx_in = modulate(hc, mod_c[0], mod_c[1])

        kv_swa_c = ctx_in @ w_in_i[:, OFF_SWA_K:OFF_CONV]
        kv_nat_c = ctx_in @ w_in_i[:, OFF_NAT_K:IN_COLS]
        k_swa_c = split_heads(kv_swa_c[..., :SWA_KV], SWA_KV_HEADS)
        v_swa_c = split_heads(kv_swa_c[..., SWA_KV:], SWA_KV_HEADS)
        k_nat_c = split_heads(kv_nat_c[..., :NAT_W], NAT_HEADS)
        v_nat_c = split_heads(kv_nat_c[..., NAT_W:], NAT_HEADS)

        u = modulate(h, sh_m, sc_m) @ w_in_i
        q_swa = apply_axial_rope(split_heads(u[..., :OFF_SWA_K], SWA_HEADS), cos, sin)
        k_swa = apply_axial_rope(split_heads(u[..., OFF_SWA_K:OFF_SWA_V], SWA_KV_HEADS), cos, sin)
        v_swa = split_heads(u[..., OFF_SWA_V:OFF_CONV], SWA_KV_HEADS)
        y_swa = window_attention(q_swa, k_swa, v_swa, k_swa_c, v_swa_c, attn_sink[i])
        y_conv = gated_short_conv(u[..., OFF_CONV:OFF_NAT_Q], conv_w[i], conv_b[i])
        y_nat = neighbourhood_attention(split_heads(u[..., OFF_NAT_Q:OFF_NAT_K], NAT_HEADS),
                                        split_heads(u[..., OFF_NAT_K:OFF_NAT_V], NAT_HEADS),
                                        split_heads(u[..., OFF_NAT_V:IN_COLS], NAT_HEADS),
                                        k_nat_c, v_nat_c, nat_rpb[i])
        mix = mix_output(y_swa, y_conv, y_nat, mix_norm_g[i], w_out[i])
        h_new = layer_norm(DEEPNORM_ALPHA * h + g_m * mix, ln1_g[i], ln1_b[i])
        ffn = hierarchical_moe(modulate(h_new, sh_f, sc_f), *moe_params)
        h_new = layer_norm(DEEPNORM_ALPHA * h_new + g_f * ffn, ln2_g[i], ln2_b[i])

        if not last:
            q_swa_c = split_heads(ctx_in @ w_in_i[:, :OFF_SWA_K], SWA_HEADS)
            conv_c = ctx_in @ w_in_i[:, OFF_CONV:OFF_NAT_Q]
            q_nat_c = split_heads(ctx_in @ w_in_i[:, OFF_NAT_Q:OFF_NAT_K], NAT_HEADS)
            yc_swa = context_attention(q_swa_c, k_swa_c, v_swa_c, attn_sink[i])
            yc_conv = gated_short_conv(conv_c, conv_w[i], conv_b[i])
            yc_nat = context_attention(q_nat_c, k_nat_c, v_nat_c, None)
            mix_c = mix_output(yc_swa, yc_conv, yc_nat, mix_norm_g[i], w_out[i])
            hc_new = layer_norm(DEEPNORM_ALPHA * hc + mod_c[2] * mix_c, ln1_g[i], ln1_b[i])
            ffn_c = hierarchical_moe(modulate(hc_new, mod_c[3], mod_c[4]), *moe_params)
            hc = layer_norm(DEEPNORM_ALPHA * hc_new + mod_c[5] * ffn_c, ln2_g[i], ln2_b[i])
        h = h_new
    return h
```

```python
import numpy as np
import ml_dtypes
import concourse.bass as bass
import concourse.mybir as mybir
from concourse.bass_utils import run_bass_kernel_spmd

F32 = mybir.dt.float32
BF16 = mybir.dt.bfloat16
AF = mybir.ActivationFunctionType
ALU = mybir.AluOpType

D = 4096
NL = 4
S = 4096
B = 4
CTX = 256
IN_COLS = 9216
ALPHA = (2.0 * NL) ** 0.25
LN_EPS_P = 1e-5 / (ALPHA * ALPHA)
RMS_EPS = 1e-6
SCL = 128.0 ** -0.5
NEGB = -30000.0

EXT = 2560
TOK = EXT + CTX
OWN = 2048 + CTX
HALO = 256


class Tile:
    def __init__(self, ap, name="", dsem=None):
        self.ap = ap
        self.name = name
        self.w = {}
        self.r = {}
        self.dsem = dsem
        self.excl = False

    def __getitem__(self, k):
        return self.ap[k]


class FW:
    def __init__(self, nc):
        self.nc = nc
        self.eng = {"pe": nc.tensor, "act": nc.scalar, "dve": nc.vector, "pool": nc.gpsimd, "sp": nc.sync}
        self.prog = {}
        self.waited = {k: {} for k in self.eng}
        self._ctx = []
        self._persist = []
        self._scopes = []
        self.allsems = []
        self.n_ins = 0
        self.uid = 0
        self.free_dsems = []
        self._scope_dsems = [[]]

    def sem(self, name):
        return self.sem_persist(name)

    def new_phase(self, tag):
        for e in ("pe", "act", "dve", "pool"):
            self.prog[e] = self.sem(f"p_{tag}_{e}")

    def push(self):
        self._scopes.append(len(self._ctx))
        self._scope_dsems.append([])

    def pop(self):
        n = self._scopes.pop()
        while len(self._ctx) > n:
            cm = self._ctx.pop()
            cm.__exit__(None, None, None)
        self.free_dsems.extend(self._scope_dsems.pop())

    def dsem_get(self):
        if self.free_dsems:
            rec = self.free_dsems.pop()
        else:
            rec = self.sem_persist(f"dq{len(self.allsems)}")
        self._scope_dsems[-1].append(rec)
        return rec

    def sem_persist(self, name):
        cm = self.nc.semaphore(name)
        h = cm.__enter__()
        self._persist.append(cm)
        rec = [h, 0, name]
        self.allsems.append(rec)
        return rec

    def sbuf(self, name, shape, dtype, dma=False):
        self.uid += 1
        name = f"{name}_{self.uid}"
        cm = self.nc.sbuf_tensor(name, shape, dtype)
        h = cm.__enter__()
        self._ctx.append(cm)
        ds = self.dsem_get() if dma else None
        return Tile(h, name, ds)

    def psum(self, name, shape, dtype=F32):
        self.uid += 1
        name = f"{name}_{self.uid}"
        cm = self.nc.psum_tensor(name, shape, dtype)
        h = cm.__enter__()
        self._ctx.append(cm)
        t = Tile(h, name)
        t.excl = True
        return t

    def dram(self, name, shape, dtype, kind="Internal"):
        h = self.nc.dram_tensor(name, shape, dtype, kind=kind)
        return Tile(h.ap(), name)

    def close(self):
        while self._ctx:
            self._ctx.pop().__exit__(None, None, None)
        while self._persist:
            self._persist.pop().__exit__(None, None, None)

    def wait(self, e, toks, skip_same=None):
        wd = self.waited[e]
        skip_nm = None
        if skip_same is not None and skip_same in self.prog:
            skip_nm = self.prog[skip_same][2]
        for nm, (s, v) in list(toks.items()):
            if nm == skip_nm:
                continue
            if wd.get(nm, 0) >= v:
                continue
            self.eng[e].wait_ge(s, v)
            wd[nm] = v

    @staticmethod
    def _merge(d, rec, v):
        nm = rec[2]
        if nm not in d or d[nm][1] < v:
            d[nm] = (rec[0], v)

    def op(self, e, emit, reads=(), writes=()):
        for t in reads:
            self.wait(e, t.w)
            if t.excl:
                self.wait(e, t.r, skip_same=e)
        for t in writes:
            self.wait(e, t.w, skip_same=e)
            self.wait(e, t.r, skip_same=e)
        ins = emit(self.eng[e])
        p = self.prog[e]
        p[1] += 1
        ins.then_inc(p[0], 1)
        self.n_ins += 1
        for t in reads:
            self._merge(t.r, p, p[1])
        for t in writes:
            self._merge(t.w, p, p[1])
        return ins

    def mm(self, out_t, emit_list, reads=()):
        e = "pe"
        for t in reads:
            self.wait(e, t.w)
        self.wait(e, out_t.w, skip_same=e)
        self.wait(e, out_t.r, skip_same=e)
        ins = None
        for f in emit_list:
            ins = f(self.eng[e])
            self.n_ins += 1
        p = self.prog[e]
        p[1] += 1
        ins.then_inc(p[0], 1)
        for t in reads:
            self._merge(t.r, p, p[1])
        self._merge(out_t.w, p, p[1])

    def dma(self, q, out_t, out_ap, in_t, in_ap, semtile=None):
        st = semtile if semtile is not None else (out_t if out_t.dsem is not None else in_t)
        ds = st.dsem
        assert ds is not None, (out_t.name, in_t.name)
        self.wait(q, in_t.w)
        self.wait(q, out_t.w)
        self.wait(q, out_t.r)
        ins = self.eng[q].dma_start(out=out_ap, in_=in_ap)
        ds[1] += 16
        ins.then_inc(ds[0], 16)
        self.n_ins += 1
        self._merge(in_t.r, ds, ds[1])
        self._merge(out_t.w, ds, ds[1])
        return ins

    def barrier(self):
        toks = {}
        for rec in self.allsems:
            if rec[1] > 0:
                toks[rec[2]] = (rec[0], rec[1])
        for e in ("sp", "pool", "act", "dve", "pe"):
            self.wait(e, toks)


def _f(fn, *a, **k):
    return lambda eng: fn(eng, *a, **k)


ADA_CS = 3072


def build_ada():
    nc = bass.Bass("TRN2", target_bir_lowering=False)
    fw = FW(nc)
    fw.new_phase("a")
    cT = fw.dram("cT", [128, 32, 8], F32, kind="ExternalInput")
    wa = fw.dram("wa", [NL, D, ADA_CS], F32, kind="ExternalInput")
    ba = fw.dram("ba", [NL, ADA_CS], F32, kind="ExternalInput")
    out = fw.dram("mod", [NL, 8, ADA_CS], F32, kind="ExternalOutput")
    CW = 256
    nct = ADA_CS // CW
    c_sb = fw.sbuf("c_sb", [128, 32, 8], F32, dma=True)
    sg = fw.sbuf("sg", [128, 32, 8], F32)
    act = fw.sbuf("actv", [128, 32, 8], F32)
    wr = [fw.sbuf(f"w{i}", [128, 32, CW], F32, dma=True) for i in range(2)]
    bb = [fw.sbuf(f"bb{i}", [8, ADA_CS], F32, dma=True) for i in range(2)]
    res = [fw.sbuf(f"res{i}", [8, ADA_CS], F32, dma=True) for i in range(2)]
    ps = [fw.psum(f"ps{i}", [8, 512]) for i in range(2)]
    fw.dma("sp", c_sb, c_sb[:], cT, cT.ap)
    fw.op("act", lambda e: e.activation(out=sg[:], in_=c_sb[:], func=AF.Sigmoid), reads=[c_sb], writes=[sg])
    fw.op("dve", lambda e: e.tensor_tensor(out=act[:], in0=sg[:], in1=c_sb[:], op=ALU.mult), reads=[sg, c_sb], writes=[act])
    i = 0
    for l in range(NL):
        b_t = bb[l % 2]
        r_t = res[l % 2]
        fw.dma("sp", b_t, b_t[:], ba, ba.ap[l:l + 1, :].broadcast_to([8, ADA_CS]))
        for ct in range(nct):
            w_t = wr[i % 2]
            p_t = ps[i % 2]
            fw.dma("sp", w_t, w_t[:], wa, wa.ap[l, :, ct * CW:(ct + 1) * CW].rearrange("(kc p) n -> p kc n", p=128))
            fw.mm(p_t, [(lambda e, kc=kc, w_t=w_t, p_t=p_t: e.matmul(p_t[:, 0:CW], lhsT=act[:, kc, :], rhs=w_t[:, kc, :],
                                                                    start=(kc == 0), stop=(kc == 31))) for kc in range(32)],
                  reads=[act, w_t])
            fw.op("dve", lambda e, p_t=p_t, r_t=r_t, b_t=b_t, ct=ct: e.tensor_tensor(
                out=r_t[:, ct * CW:(ct + 1) * CW], in0=p_t[:, 0:CW], in1=b_t[:, ct * CW:(ct + 1) * CW], op=ALU.add),
                reads=[p_t, b_t], writes=[r_t])
            i += 1
        fw.dma("sp", out, out.ap[l], r_t, r_t[:])
    fw.wait("sp", out.w)
    fw.close()
    return nc


def own_segments(t0, n):
    segs = []
    a, b = max(t0, HALO), min(t0 + n, HALO + 2048)
    if a < b:
        segs.append((a, b - a, a - HALO))
    a, b = max(t0, EXT), min(t0 + n, TOK)
    if a < b:
        segs.append((a, b - a, a - EXT + 2048))
    return segs


def own_to_tok(o0, n):
    segs = []
    a, b = max(o0, 0), min(o0 + n, 2048)
    if a < b:
        segs.append((a, b - a, a + HALO))
    a, b = max(o0, 2048), min(o0 + n, OWN)
    if a < b:
        segs.append((a, b - a, a - 2048 + EXT))
    return segs


def build_layer(phases=("p1", "p2a", "p2b", "p2c", "p3", "p4", "p5"), dbg=False):
    nc = bass.Bass("TRN2", target_bir_lowering=False)
    fw = FW(nc)
    EI = "ExternalInput"
    hT = fw.dram("hT", [32, 128, TOK], F32, kind=EI)
    modv = fw.dram("modv", [128, 12, 32], F32, kind=EI)
    w_in = fw.dram("w_in", [D, IN_COLS], F32, kind=EI)
    w_out = fw.dram("w_out", [D, D], F32, kind=EI)
    w_gate = fw.dram("w_gate", [16, D, 256], F32, kind=EI)
    w_up = fw.dram("w_up", [16, D, 256], F32, kind=EI)
    w_down = fw.dram("w_down", [D, D], F32, kind=EI)
    w_r = fw.dram("w_r", [D, 20], F32, kind=EI)
    b_r = fw.dram("b_r", [1, 20], F32, kind=EI)
    convp = fw.dram("convp", [128, 8, 4], F32, kind=EI)
    sink = fw.dram("sink", [1, 16], F32, kind=EI)
    vecs = fw.dram("vecs", [128, 5, 32], F32, kind=EI)
    rope = fw.dram("rope", [2, 128, TOK], F32, kind=EI)
    perm = fw.dram("perm", [128, 128], BF16, kind=EI)
    ident = fw.dram("ident", [128, 128], F32, kind=EI)
    natb = fw.dram("natb", [8, 128, 8, 512], F32, kind=EI)
    natm = fw.dram("natm", [128, 4, 8, 512], BF16, kind=EI)
    kvb = fw.dram("kvb", [128, 22], F32, kind=EI)
    cedge = fw.dram("cedge", [128, 2], F32, kind=EI)
    swam = fw.dram("swam", [128, 2, 512], BF16, kind=EI)
    sel = fw.dram("sel", [16, 16, 128], F32, kind=EI)
    hout = fw.dram("hout", [32, 128, OWN], F32, kind="ExternalOutput")

    QsT = fw.dram("QsT", [16, 128, OWN], BF16)
    KsT = fw.dram("KsT", [4, 128, TOK], BF16)
    Vs = fw.dram("Vs", [22, 128, 512], BF16)
    QnT = fw.dram("QnT", [8, 128, OWN], BF16)
    KnT = fw.dram("KnT", [8, 128, TOK], BF16)
    Vn = fw.dram("Vn", [22, 128, 1024], BF16)
    cvT = fw.dram("cvT", [24, 128, TOK], F32)
    yT = fw.dram("yT", [32, 128, OWN], BF16)
    z1T = fw.dram("z1T", [32, 128, OWN], F32)
    h1T = fw.dram("h1T", [32, 128, OWN], F32)
    z2T = fw.dram("z2T", [32, 128, OWN], F32)
    st1 = fw.dram("st1", [2, 128, OWN], F32)
    st2 = fw.dram("st2", [2, 128, OWN], F32)
    dbg_t = {}

    fw.new_phase("c")
    mo = fw.sbuf("mo", [128, 12, 32], F32, dma=True)
    vc = fw.sbuf("vc", [128, 5, 32], F32, dma=True)
    kvb_s = fw.sbuf("kvb_s", [128, 22], F32, dma=True)
    ones_b = fw.sbuf("ones_b", [128, 128], BF16)
    ones_f = fw.sbuf("ones_f", [128, 128], F32)
    fw.dma("sp", mo, mo[:], modv, modv.ap)
    fw.dma("sp", vc, vc[:], vecs, vecs.ap)
    fw.dma("sp", kvb_s, kvb_s[:], kvb, kvb.ap)
    fw.op("dve", lambda e: e.memset(ones_b[:], 1.0), writes=[ones_b])
    fw.op("dve", lambda e: e.memset(ones_f[:], 1.0), writes=[ones_f])
    for m in (1, 4, 7, 10):
        fw.op("dve", lambda e, m=m: e.tensor_scalar(out=mo[:, m, :], in0=mo[:, m, :], scalar1=1.0, scalar2=None, op0=ALU.add),
              reads=[mo], writes=[mo])
    for m in (2, 5, 8, 11):
        fw.op("dve", lambda e, m=m: e.tensor_scalar(out=mo[:, m, :], in0=mo[:, m, :], scalar1=1.0 / ALPHA, scalar2=None, op0=ALU.mult),
              reads=[mo], writes=[mo])
    fw.barrier()

    def MO(m, ctx, kc):
        mm_ = m + (6 if ctx else 0)
        return mo[:, mm_, kc:kc + 1]

    if "p1" in phases:
        fw.new_phase("p1")
        fw.push()
        SBT = 1408
        WT = 256
        xm = fw.sbuf("xm", [128, 32, SBT], BF16)
        hst = [fw.sbuf(f"hst{i}", [128, 2, SBT], F32, dma=True) for i in range(2)]
        wt = [fw.sbuf(f"wt{i}", [128, 32, WT], BF16, dma=True) for i in range(3)]
        rp = fw.sbuf("rp", [128, 2, SBT], F32, dma=True)
        pm = fw.sbuf("pm", [128, 128], BF16, dma=True)
        xb = [fw.sbuf(f"xb{i}", [128, 512], BF16) for i in range(2)]
        t1 = [fw.sbuf(f"t1{i}", [128, 512], F32) for i in range(2)]
        t2 = [fw.sbuf(f"t2{i}", [128, 512], F32) for i in range(2)]
        s16 = [fw.sbuf(f"s16{i}", [128, 512], BF16, dma=True) for i in range(4)]
        s32 = [fw.sbuf(f"s32{i}", [128, 512], F32, dma=True) for i in range(3)]
        pA = [fw.psum(f"pA{i}", [128, 512]) for i in range(4)]
        pB = [fw.psum(f"pB{i}", [128, 512]) for i in range(2)]
        fw.dma("sp", pm, pm[:], perm, perm.ap)
        cnt = {"a": 0, "b": 0, "s16": 0, "s32": 0, "r": 0, "w": 0, "h": 0}
        deferred = []

        def run_deferred():
            while deferred:
                deferred.pop(0)()

        wtiles = []
        for i in range(8):
            wtiles.append((i * WT, "ropeq", i))
        for i in range(2):
            wtiles.append((2048 + i * WT, "ropek", i))
        for i in range(2):
            wtiles.append((2560 + i * WT, "vs", i))
        for i in range(12):
            wtiles.append((3072 + i * WT, "conv", i))
        for i in range(4):
            wtiles.append((6144 + i * WT, "natq", i))
        for i in range(4):
            wtiles.append((7168 + i * WT, "natk", i))
        for i in range(4):
            wtiles.append((8192 + i * WT, "vn", i))

        import os as _os
        _kinds = _os.environ.get("P1_KINDS")
        if _kinds:
            wtiles = [w for w in wtiles if w[1] in _kinds.split(",")]
        _nw = int(_os.environ.get("P1_NW", "0"))
        if _nw:
            wtiles = wtiles[:_nw]
        for sb in range(int(_os.environ.get("P1_SB", "2"))):
            T0 = sb * SBT
            fw.dma("sp", rp, rp[:], rope, rope.ap[:, :, T0:T0 + SBT].rearrange("c p t -> p c t"))
            for pc in range(16):
                h_t = hst[cnt["h"] % 2]
                cnt["h"] += 1
                fw.dma("sp", h_t, h_t[:], hT, hT.ap[pc * 2:(pc + 1) * 2, :, T0:T0 + SBT].rearrange("kc p t -> p kc t"))
                for kk in range(2):
                    kc = pc * 2 + kk
                    parts = [(0, SBT, False)] if sb == 0 else [(0, EXT - T0, False), (EXT - T0, SBT, True)]
                    for (a, b_, isc) in parts:
                        if kc % 2 == 0:
                            fw.op("act", lambda e, h_t=h_t, kk=kk, kc=kc, a=a, b_=b_, isc=isc: e.activation(
                                out=xm[:, kc, a:b_], in_=h_t[:, kk, a:b_], func=AF.Identity,
                                scale=MO(1, isc, kc), bias=MO(0, isc, kc)), reads=[h_t, mo], writes=[xm])
                        else:
                            fw.op("dve", lambda e, h_t=h_t, kk=kk, kc=kc, a=a, b_=b_, isc=isc: e.tensor_scalar(
                                out=xm[:, kc, a:b_], in0=h_t[:, kk, a:b_], scalar1=MO(1, isc, kc), scalar2=MO(0, isc, kc),
                                op0=ALU.mult, op1=ALU.add), reads=[h_t, mo], writes=[xm])
            chunks = [(0, 512), (512, 512), (1024, 384)]
            for (c0, kind, info) in wtiles:
                w_t = wt[cnt["w"] % 3]
                cnt["w"] += 1
                fw.dma("pool", w_t, w_t[:], w_in, w_in.ap[:, c0:c0 + WT].rearrange("(kc p) n -> p kc n", p=128))
                if kind in ("vs", "vn"):
                    for tt in range(11):
                        A = pA[cnt["a"] % 4]
                        cnt["a"] += 1
                        fw.mm(A, [(lambda e, kc=kc, A=A, tt=tt, w_t=w_t: e.matmul(
                            A[:, 0:WT], lhsT=xm[:, kc, tt * 128:(tt + 1) * 128], rhs=w_t[:, kc, :],
                            start=(kc == 0), stop=(kc == 31))) for kc in range(32)], reads=[xm, w_t])
                        run_deferred()
                        st = s16[cnt["s16"] % 4]
                        cnt["s16"] += 1
                        if tt % 2 == 0:
                            fw.op("act", lambda e, st=st, A=A: e.copy(out=st[:, 0:WT], in_=A[:, 0:WT]), reads=[A], writes=[st])
                        else:
                            fw.op("dve", lambda e, st=st, A=A: e.tensor_copy(out=st[:, 0:WT], in_=A[:, 0:WT]), reads=[A], writes=[st])
                        gt = sb * 11 + tt
                        if kind == "vs":
                            fw.dma("sp", Vs, Vs.ap[gt][:, info * WT:(info + 1) * WT], st, st[:, 0:WT])
                        else:
                            fw.dma("sp", Vn, Vn.ap[gt][:, info * WT:(info + 1) * WT], st, st[:, 0:WT])
                    continue
                for sub in range(WT // 128):
                    for (q0, n) in chunks:
                        A = pA[cnt["a"] % 4]
                        cnt["a"] += 1
                        fw.mm(A, [(lambda e, kc=kc, A=A, sub=sub, q0=q0, n=n, w_t=w_t: e.matmul(
                            A[:, 0:n], lhsT=w_t[:, kc, sub * 128:(sub + 1) * 128], rhs=xm[:, kc, q0:q0 + n],
                            start=(kc == 0), stop=(kc == 31))) for kc in range(32)], reads=[xm, w_t])
                        run_deferred()
                        tg = T0 + q0
                        idx2 = info * (WT // 128) + sub
                        if kind in ("ropeq", "ropek"):
                            r = cnt["r"] % 2
                            cnt["r"] += 1
                            xb_t, t1_t, t2_t = xb[r], t1[r], t2[r]
                            Bp = pB[r]
                            fw.op("act", lambda e, xb_t=xb_t, A=A, n=n: e.copy(out=xb_t[:, 0:n], in_=A[:, 0:n]),
                                  reads=[A], writes=[xb_t])
                            fw.op("dve", lambda e, t1_t=t1_t, A=A, n=n, q0=q0: e.tensor_tensor(
                                out=t1_t[:, 0:n], in0=A[:, 0:n], in1=rp[:, 0, q0:q0 + n], op=ALU.mult),
                                reads=[A, rp], writes=[t1_t])

                            def part2(xb_t=xb_t, t1_t=t1_t, t2_t=t2_t, Bp=Bp, n=n, tg=tg, q0=q0, kind=kind, idx2=idx2):
                                _stg = int(_os.environ.get("ROPE_STAGE", "3"))
                                if _stg >= 2:
                                    fw.mm(Bp, [lambda e: e.matmul(Bp[:, 0:n], lhsT=pm[:], rhs=xb_t[:, 0:n], start=True, stop=True)],
                                          reads=[pm, xb_t])
                                if _stg >= 3:
                                    fw.op("dve", lambda e: e.tensor_tensor(out=t2_t[:, 0:n], in0=Bp[:, 0:n], in1=rp[:, 1, q0:q0 + n],
                                                                           op=ALU.mult), reads=[Bp, rp], writes=[t2_t])
                                st = s16[cnt["s16"] % 4]
                                cnt["s16"] += 1
                                if _stg >= 3:
                                    fw.op("dve", lambda e: e.tensor_tensor(out=st[:, 0:n], in0=t1_t[:, 0:n], in1=t2_t[:, 0:n], op=ALU.add),
                                          reads=[t1_t, t2_t], writes=[st])
                                else:
                                    fw.op("dve", lambda e: e.tensor_copy(out=st[:, 0:n], in_=t1_t[:, 0:n]), reads=[t1_t], writes=[st])
                                if kind == "ropek":
                                    fw.dma("sp", KsT, KsT.ap[idx2][:, tg:tg + n], st, st[:, 0:n])
                                else:
                                    for (ts, ln, os_) in own_segments(tg, n):
                                        fw.dma("sp", QsT, QsT.ap[idx2][:, os_:os_ + ln], st, st[:, ts - tg:ts - tg + ln])
                            deferred.append(part2)
                        elif kind in ("natq", "natk"):
                            st = s16[cnt["s16"] % 4]
                            cnt["s16"] += 1
                            fw.op("act", lambda e, st=st, A=A, n=n: e.copy(out=st[:, 0:n], in_=A[:, 0:n]), reads=[A], writes=[st])
                            if kind == "natk":
                                fw.dma("sp", KnT, KnT.ap[idx2][:, tg:tg + n], st, st[:, 0:n])
                            else:
                                for (ts, ln, os_) in own_segments(tg, n):
                                    fw.dma("sp", QnT, QnT.ap[idx2][:, os_:os_ + ln], st, st[:, ts - tg:ts - tg + ln])
                        else:
                            st = s32[cnt["s32"] % 3]
                            cnt["s32"] += 1
                            fw.op("act", lambda e, st=st, A=A, n=n: e.copy(out=st[:, 0:n], in_=A[:, 0:n]), reads=[A], writes=[st])
                            fw.dma("sp", cvT, cvT.ap[idx2][:, tg:tg + n], st, st[:, 0:n])
            run_deferred()
        fw.barrier()
        fw.pop()

    def attention_block(rhsQ, n_free, keys, Kt, Vt, pS, pO, pDn, Pb, cnt, masks, finalize):
        O = pO[cnt["o"] % 2]
        Dn = pDn[cnt["o"] % 2]
        cnt["o"] += 1
        pend = []
        nk = len(keys)
        for idx, (kt, mlist) in enumerate(keys):
            S_ = pS[cnt["s"] % len(pS)]
            P_ = Pb[cnt["s"] % len(Pb)]
            cnt["s"] += 1
            fw.mm(S_, [lambda e, S_=S_, kt=kt: e.matmul(S_[:, 0:n_free], lhsT=Kt[:, kt * 128:(kt + 1) * 128], rhs=rhsQ,
                                                        start=True, stop=True)], reads=[Kt] + masks["qtiles"])
            while pend:
                pend.pop(0)()
            fw.op("act", lambda e, S_=S_, P_=P_, kt=kt: e.activation(out=P_[:, 0:n_free], in_=S_[:, 0:n_free], func=AF.Exp,
                                                                     scale=SCL, bias=kvb_s[:, kt:kt + 1]),
                  reads=[S_, kvb_s], writes=[P_])
            for (mt, map_) in mlist:
                fw.op("dve", lambda e, P_=P_, map_=map_: e.tensor_tensor(out=P_[:, 0:n_free], in0=P_[:, 0:n_free], in1=map_, op=ALU.mult),
                      reads=[P_, mt], writes=[P_])

            def pv(P_=P_, kt=kt, idx=idx):
                fw.mm(O, [lambda e: e.matmul(O[:, 0:n_free], lhsT=Vt[:, kt, :], rhs=P_[:, 0:n_free], start=(idx == 0), stop=(idx == nk - 1))],
                      reads=[Vt, P_])
                fw.mm(Dn, [lambda e: e.matmul(Dn[:, 0:n_free], lhsT=ones_b[:], rhs=P_[:, 0:n_free], start=(idx == 0), stop=(idx == nk - 1))],
                      reads=[ones_b, P_])
            pend.append(pv)
        while pend:
            pend.pop(0)()
        finalize(O, Dn)

    if "p2a" in phases or "p2b" in phases:
        fw.new_phase("p2")
        fw.push()
        pS = [fw.psum(f"pS{i}", [128, 512]) for i in range(4)]
        pO = [fw.psum(f"pO{i}", [128, 512]) for i in range(2)]
        pDn = [fw.psum(f"pDn{i}", [128, 512]) for i in range(2)]
        Pb = [fw.sbuf(f"Pb{i}", [128, 512], BF16) for i in range(4)]
        dsb = [fw.sbuf(f"dsb{i}", [128, 512], F32) for i in range(2)]
        cnt = {"s": 0, "o": 0, "d": 0}

    if "p2a" in phases:
        fw.push()
        sm = fw.sbuf("sm", [128, 2, 512], BF16, dma=True)
        es = fw.sbuf("es", [128, 16], F32, dma=True)
        esx = fw.sbuf("esx", [128, 16, 128], F32)
        Kg = [fw.sbuf(f"Kg{i}", [128, TOK], BF16, dma=True) for i in range(2)]
        Vg = [fw.sbuf(f"Vg{i}", [128, 22, 128], BF16, dma=True) for i in range(2)]
        Qg = [fw.sbuf(f"Qg{i}", [128, 4, OWN], BF16, dma=True) for i in range(2)]
        yst = [fw.sbuf(f"yst{i}", [128, 4, OWN], BF16, dma=True) for i in range(2)]
        fw.dma("sp", sm, sm[:], swam, swam.ap)
        fw.dma("sp", es, es[:], sink, sink.ap.broadcast_to([128, 16]))
        fw.op("act", lambda e: e.activation(out=es[:], in_=es[:], func=AF.Exp), reads=[es], writes=[es])
        fw.op("dve", lambda e: e.tensor_copy(out=esx[:], in_=es[:, :].unsqueeze(2).broadcast_to([128, 16, 128])), reads=[es], writes=[esx])
        for g in range(4):
            K_t, V_t, Q_t, y_t = Kg[g % 2], Vg[g % 2], Qg[g % 2], yst[g % 2]
            fw.dma("sp", K_t, K_t[:], KsT, KsT.ap[g])
            fw.dma("sp", V_t, V_t[:], Vs, Vs.ap[:, :, g * 128:(g + 1) * 128].rearrange("t k d -> k t d"))
            fw.dma("sp", Q_t, Q_t[:], QsT, QsT.ap[4 * g:4 * g + 4].rearrange("h d o -> d h o"))
            for blk in range(18):
                if blk < 16:
                    qt = blk + 2
                    keys = [(qt - 1, [(sm, sm[:, 0, :])]), (qt, []), (qt + 1, [(sm, sm[:, 1, :])]), (20, []), (21, [])]
                    oc = blk * 128
                else:
                    keys = [(20, []), (21, [])]
                    oc = 2048 + (blk - 16) * 128
                rhsQ = Q_t[:, :, oc:oc + 128]

                def fin(O, Dn, g=g, oc=oc, y_t=y_t):
                    d_ = dsb[cnt["d"] % 2]
                    cnt["d"] += 1
                    fw.op("dve", lambda e: e.tensor_tensor(out=d_[:].rearrange("p (h q) -> p h q", h=4), in0=Dn[:].rearrange("p (h q) -> p h q", h=4),
                                                           in1=esx[:, 4 * g:4 * g + 4, :], op=ALU.add), reads=[Dn, esx], writes=[d_])
                    fw.op("dve", lambda e: e.reciprocal(out=d_[:], in_=d_[:]), reads=[d_], writes=[d_])
                    fw.op("dve", lambda e: e.tensor_tensor(out=y_t[:, :, oc:oc + 128], in0=O[:].rearrange("p (h q) -> p h q", h=4),
                                                           in1=d_[:].rearrange("p (h q) -> p h q", h=4), op=ALU.mult), reads=[O, d_], writes=[y_t])
                attention_block_swa(fw, rhsQ, keys, K_t, V_t, Q_t, pS, pO, pDn, Pb, cnt, kvb_s, ones_b, fin)
            fw.dma("sp", yT, yT.ap[4 * g:4 * g + 4].rearrange("c p o -> p c o"), y_t, y_t[:])
        fw.barrier()
        fw.pop()

    if "p2b" in phases:
        fw.push()
        nm = fw.sbuf("nm", [128, 4, 8, 512], BF16, dma=True)
        nbf = [fw.sbuf(f"nbf{i}", [128, 8, 512], F32, dma=True) for i in range(1)]
        Eh = [fw.sbuf(f"Eh{i}", [128, 8, 512], BF16) for i in range(2)]
        Kh = [fw.sbuf(f"Kh{i}", [128, TOK], BF16, dma=True) for i in range(2)]
        Vh = [fw.sbuf(f"Vh{i}", [128, 22, 128], BF16, dma=True) for i in range(2)]
        Qh = [fw.sbuf(f"Qh{i}", [128, OWN], BF16, dma=True) for i in range(2)]
        ysh = [fw.sbuf(f"ysh{i}", [128, OWN], BF16, dma=True) for i in range(2)]
        fw.dma("sp", nm, nm[:], natm, natm.ap)
        for h in range(8):
            K_t, V_t, Q_t, y_t, E_t = Kh[h % 2], Vh[h % 2], Qh[h % 2], ysh[h % 2], Eh[h % 2]
            nb_t = nbf[0]
            fw.dma("sp", K_t, K_t[:], KnT, KnT.ap[h])
            fw.dma("sp", V_t, V_t[:], Vn, Vn.ap[:, :, h * 128:(h + 1) * 128].rearrange("t k d -> k t d"))
            fw.dma("sp", Q_t, Q_t[:], QnT, QnT.ap[h])
            fw.dma("sp", nb_t, nb_t[:], natb, natb.ap[h])
            fw.op("act", lambda e, E_t=E_t, nb_t=nb_t: e.activation(out=E_t[:], in_=nb_t[:], func=AF.Exp), reads=[nb_t], writes=[E_t])
            for blk in range(5):
                if blk < 4:
                    keys = [(4 * blk + j, [(E_t, E_t[:, j, :]), (nm, nm[:, blk, j, :])]) for j in range(8)] + [(20, []), (21, [])]
                    oc, nf = blk * 512, 512
                else:
                    keys = [(20, []), (21, [])]
                    oc, nf = 2048, 256
                rhsQ = Q_t[:, oc:oc + nf]

                def fin(O, Dn, oc=oc, nf=nf, y_t=y_t):
                    d_ = dsb[cnt["d"] % 2]
                    cnt["d"] += 1
                    fw.op("dve", lambda e: e.reciprocal(out=d_[:, 0:nf], in_=Dn[:, 0:nf]), reads=[Dn], writes=[d_])
                    fw.op("dve", lambda e: e.tensor_tensor(out=y_t[:, oc:oc + nf], in0=O[:, 0:nf], in1=d_[:, 0:nf], op=ALU.mult),
                          reads=[O, d_], writes=[y_t])
                attention_block(rhsQ, nf, keys, K_t, V_t, pS, pO, pDn, Pb, cnt, {"qtiles": [Q_t]}, fin)
            fw.dma("sp", yT, yT.ap[24 + h], y_t, y_t[:])
        fw.barrier()
        fw.pop()

    if "p2a" in phases or "p2b" in phases:
        fw.barrier()
        fw.pop()

    if "p2c" in phases:
        fw.new_phase("p2c")
        fw.push()
        cp = fw.sbuf("cp", [128, 8, 4], F32, dma=True)
        ce = fw.sbuf("ce", [128, 2], F32, dma=True)
        ub = [fw.sbuf(f"ub{i}", [128, TOK], F32, dma=True) for i in range(2)]
        cb = [fw.sbuf(f"cb{i}", [128, TOK], F32, dma=True) for i in range(2)]
        bbuf = [fw.sbuf(f"bbuf{i}", [128, TOK], F32, dma=True) for i in range(2)]
        acc = [fw.sbuf(f"acc{i}", [128, OWN], F32) for i in range(1)]
        ycv = [fw.sbuf(f"ycv{i}", [128, OWN], BF16, dma=True) for i in range(2)]
        fw.dma("sp", cp, cp[:], convp, convp.ap)
        fw.dma("sp", ce, ce[:], cedge, cedge.ap)
        for cc in range(8):
            u_t, c_t, b_t, a_t, y_t = ub[cc % 2], cb[cc % 2], bbuf[cc % 2], acc[0], ycv[cc % 2]
            fw.dma("sp", u_t, u_t[:], cvT, cvT.ap[cc])
            fw.dma("sp", b_t, b_t[:], cvT, cvT.ap[8 + cc])
            fw.dma("sp", c_t, c_t[:], cvT, cvT.ap[16 + cc])
            fw.op("dve", lambda e: e.tensor_tensor(out=u_t[:], in0=u_t[:], in1=c_t[:], op=ALU.mult), reads=[u_t, c_t], writes=[u_t])
            fw.op("dve", lambda e: e.tensor_scalar(out=u_t[:, HALO - 1:HALO], in0=u_t[:, HALO - 1:HALO], scalar1=ce[:, 0:1], scalar2=None, op0=ALU.mult),
                  reads=[u_t, ce], writes=[u_t])
            fw.op("dve", lambda e: e.tensor_scalar(out=u_t[:, HALO + 2048:HALO + 2049], in0=u_t[:, HALO + 2048:HALO + 2049], scalar1=ce[:, 1:2],
                                                   scalar2=None, op0=ALU.mult), reads=[u_t, ce], writes=[u_t])
            w0, w1, w2, bb_ = cp[:, cc, 0:1], cp[:, cc, 1:2], cp[:, cc, 2:3], cp[:, cc, 3:4]
            L0, L1 = HALO, HALO + 2048
            fw.op("act", lambda e: e.activation(out=a_t[:, 0:2048], in_=u_t[:, L0:L1], func=AF.Identity, scale=w1, bias=bb_),
                  reads=[u_t, cp], writes=[a_t])
            fw.op("dve", lambda e: e.scalar_tensor_tensor(out=a_t[:, 0:2048], in0=u_t[:, L0 - 1:L1 - 1], scalar=w0, in1=a_t[:, 0:2048],
                                                          op0=ALU.mult, op1=ALU.add), reads=[u_t, a_t, cp], writes=[a_t])
            fw.op("dve", lambda e: e.scalar_tensor_tensor(out=a_t[:, 0:2048], in0=u_t[:, L0 + 1:L1 + 1], scalar=w2, in1=a_t[:, 0:2048],
                                                          op0=ALU.mult, op1=ALU.add), reads=[u_t, a_t, cp], writes=[a_t])
            C0, C1 = EXT, TOK
            fw.op("act", lambda e: e.activation(out=a_t[:, 2048:OWN], in_=u_t[:, C0:C1], func=AF.Identity, scale=w1, bias=bb_),
                  reads=[u_t, cp], writes=[a_t])
            fw.op("dve", lambda e: e.scalar_tensor_tensor(out=a_t[:, 2049:OWN], in0=u_t[:, C0:C1 - 1], scalar=w0, in1=a_t[:, 2049:OWN],
                                                          op0=ALU.mult, op1=ALU.add), reads=[u_t, a_t, cp], writes=[a_t])
            fw.op("dve", lambda e: e.scalar_tensor_tensor(out=a_t[:, 2048:OWN - 1], in0=u_t[:, C0 + 1:C1], scalar=w2, in1=a_t[:, 2048:OWN - 1],
                                                          op0=ALU.mult, op1=ALU.add), reads=[u_t, a_t, cp], writes=[a_t])
            fw.op("dve", lambda e: e.tensor_tensor(out=y_t[:, 0:2048], in0=a_t[:, 0:2048], in1=b_t[:, L0:L1], op=ALU.mult),
                  reads=[a_t, b_t], writes=[y_t])
            fw.op("dve", lambda e: e.tensor_tensor(out=y_t[:, 2048:OWN], in0=a_t[:, 2048:OWN], in1=b_t[:, C0:C1], op=ALU.mult),
                  reads=[a_t, b_t], writes=[y_t])
            fw.dma("sp", yT, yT.ap[16 + cc], y_t, y_t[:])
        fw.barrier()
        fw.pop()

    def ln_finalize(s1, s2, chunks, o_base, st_dram, tmp_a, tmp_b):
        for ci, (q0, n, isc) in enumerate(chunks):
            mean, rstd = tmp_a[ci % len(tmp_a)], tmp_b[ci % len(tmp_b)]
            fw.op("act", lambda e: e.mul(out=mean[:, 0:n], in_=s1[ci][:, 0:n], mul=1.0 / D), reads=[s1[ci]], writes=[mean])
            fw.op("dve", lambda e: e.tensor_tensor(out=rstd[:, 0:n], in0=mean[:, 0:n], in1=mean[:, 0:n], op=ALU.mult), reads=[mean], writes=[rstd])
            fw.op("dve", lambda e: e.scalar_tensor_tensor(out=rstd[:, 0:n], in0=s2[ci][:, 0:n], scalar=1.0 / D, in1=rstd[:, 0:n],
                                                          op0=ALU.mult, op1=ALU.subtract), reads=[s2[ci], rstd], writes=[rstd])
            fw.op("dve", lambda e: e.tensor_scalar(out=rstd[:, 0:n], in0=rstd[:, 0:n], scalar1=LN_EPS_P, scalar2=None, op0=ALU.add),
                  reads=[rstd], writes=[rstd])
            fw.op("dve", lambda e: e.reciprocal(out=rstd[:, 0:n], in_=rstd[:, 0:n]), reads=[rstd], writes=[rstd])
            fw.op("act", lambda e: e.activation(out=rstd[:, 0:n], in_=rstd[:, 0:n], func=AF.Sqrt), reads=[rstd], writes=[rstd])
            fw.dma("sp", st_dram, st_dram.ap[0][:, o_base + q0:o_base + q0 + n], mean, mean[:, 0:n])
            fw.dma("sp", st_dram, st_dram.ap[1][:, o_base + q0:o_base + q0 + n], rstd, rstd[:, 0:n])

    if "p3" in phases:
        fw.new_phase("p3")
        fw.push()
        SB3 = 1152
        ySB = fw.sbuf("ySB", [128, 32, SB3], BF16, dma=True)
        rinv = fw.sbuf("rinv", [128, 3, SB3], F32)
        sq = [fw.sbuf(f"sq{i}", [128, 512], BF16) for i in range(3)]
        wt = [fw.sbuf(f"wo{i}", [128, 32, 512], BF16, dma=True) for i in range(2)]
        hres = [fw.sbuf(f"hres{i}", [128, SB3], F32, dma=True) for i in range(2)]
        zt = [fw.sbuf(f"zt{i}", [128, 512], F32, dma=True) for i in range(3)]
        zq = [fw.sbuf(f"zq{i}", [128, 512], F32) for i in range(3)]
        mtmp = [fw.sbuf(f"mtmp{i}", [128, 512], F32, dma=True) for i in range(2)]
        rtmp = [fw.sbuf(f"rtmp{i}", [128, 512], F32, dma=True) for i in range(2)]
        pA = [fw.psum(f"p3A{i}", [128, 512]) for i in range(2)]
        ps1 = [fw.psum(f"p3s1{i}", [128, 512]) for i in range(3)]
        ps2 = [fw.psum(f"p3s2{i}", [128, 512]) for i in range(3)]
        cnt = {"a": 0, "w": 0, "h": 0, "z": 0, "q": 0}
        groups = [(0, 16, 2048.0), (16, 24, 1024.0), (24, 32, 1024.0)]
        for sb in range(2):
            O0 = sb * SB3
            chunks = [(0, 512, False), (512, 512, False), (1024, 128, False)] if sb == 0 else \
                     [(0, 512, False), (512, 384, False), (896, 256, True)]
            for q4 in range(4):
                fw.dma("sp", ySB, ySB[:, q4 * 8:(q4 + 1) * 8, :], yT, yT.ap[q4 * 8:(q4 + 1) * 8, :, O0:O0 + SB3].rearrange("c p o -> p c o"))
            for gi, (c0, c1, width) in enumerate(groups):
                for (q0, n, isc) in chunks:
                    A = pA[cnt["a"] % 2]
                    cnt["a"] += 1
                    for c in range(c0, c1):
                        s_t = sq[cnt["q"] % 3]
                        cnt["q"] += 1
                        fw.op("act", lambda e, s_t=s_t, c=c, q0=q0, n=n: e.activation(out=s_t[:, 0:n], in_=ySB[:, c, q0:q0 + n], func=AF.Square),
                              reads=[ySB], writes=[s_t])
                        fw.mm(A, [lambda e, s_t=s_t, c=c, A=A, n=n, c0=c0, c1=c1: e.matmul(A[:, 0:n], lhsT=ones_b[:], rhs=s_t[:, 0:n],
                                                                                         start=(c == c0), stop=(c == c1 - 1))],
                              reads=[s_t, ones_b])
                    fw.op("dve", lambda e, A=A, gi=gi, q0=q0, n=n, width=width: e.tensor_scalar(
                        out=rinv[:, gi, q0:q0 + n], in0=A[:, 0:n], scalar1=1.0 / width, scalar2=RMS_EPS, op0=ALU.mult, op1=ALU.add),
                        reads=[A], writes=[rinv])
                    fw.op("dve", lambda e, gi=gi, q0=q0, n=n: e.reciprocal(out=rinv[:, gi, q0:q0 + n], in_=rinv[:, gi, q0:q0 + n]),
                          reads=[rinv], writes=[rinv])
                    fw.op("act", lambda e, gi=gi, q0=q0, n=n: e.activation(out=rinv[:, gi, q0:q0 + n], in_=rinv[:, gi, q0:q0 + n], func=AF.Sqrt),
                          reads=[rinv], writes=[rinv])
            for gi, (c0, c1, width) in enumerate(groups):
                for c in range(c0, c1):
                    fw.op("dve", lambda e, c=c, gi=gi: e.scalar_tensor_tensor(
                        out=ySB[:, c, :], in0=ySB[:, c, :], scalar=vc[:, 0, c:c + 1], in1=rinv[:, gi, :], op0=ALU.mult, op1=ALU.mult),
                        reads=[ySB, rinv, vc], writes=[ySB])
            pend = []
            for ct in range(8):
                w_t = wt[cnt["w"] % 2]
                cnt["w"] += 1
                fw.dma("pool", w_t, w_t[:], w_out, w_out.ap[:, ct * 512:(ct + 1) * 512].rearrange("(kc p) n -> p kc n", p=128))
                for sub in range(4):
                    dc = ct * 4 + sub
                    h_t = hres[cnt["h"] % 2]
                    cnt["h"] += 1
                    for (os_, ln, ts) in own_to_tok(O0, SB3):
                        fw.dma("sp", h_t, h_t[:, os_ - O0:os_ - O0 + ln], hT, hT.ap[dc][:, ts:ts + ln])
                    for ci, (q0, n, isc) in enumerate(chunks):
                        A = pA[cnt["a"] % 2]
                        cnt["a"] += 1
                        fw.mm(A, [(lambda e, kc=kc, A=A, sub=sub, q0=q0, n=n, w_t=w_t: e.matmul(
                            A[:, 0:n], lhsT=w_t[:, kc, sub * 128:(sub + 1) * 128], rhs=ySB[:, kc, q0:q0 + n],
                            start=(kc == 0), stop=(kc == 31))) for kc in range(32)], reads=[ySB, w_t])
                        while pend:
                            pend.pop(0)()
                        z_t = zt[cnt["z"] % 3]
                        q_t = zq[cnt["z"] % 3]
                        cnt["z"] += 1
                        fw.op("dve", lambda e, z_t=z_t, A=A, dc=dc, isc=isc, h_t=h_t, q0=q0, n=n: e.scalar_tensor_tensor(
                            out=z_t[:, 0:n], in0=A[:, 0:n], scalar=MO(2, isc, dc), in1=h_t[:, q0:q0 + n], op0=ALU.mult, op1=ALU.add),
                            reads=[A, h_t, mo], writes=[z_t])
                        fw.op("act", lambda e, z_t=z_t, q_t=q_t, n=n: e.activation(out=q_t[:, 0:n], in_=z_t[:, 0:n], func=AF.Square),
                              reads=[z_t], writes=[q_t])
                        fw.dma("sp", z1T, z1T.ap[dc][:, O0 + q0:O0 + q0 + n], z_t, z_t[:, 0:n])

                        def stats(z_t=z_t, q_t=q_t, ci=ci, n=n, dc=dc):
                            fw.mm(ps1[ci], [lambda e: e.matmul(ps1[ci][:, 0:n], lhsT=ones_f[:], rhs=z_t[:, 0:n], start=(dc == 0), stop=(dc == 31))],
                                  reads=[z_t, ones_f])
                            fw.mm(ps2[ci], [lambda e: e.matmul(ps2[ci][:, 0:n], lhsT=ones_f[:], rhs=q_t[:, 0:n], start=(dc == 0), stop=(dc == 31))],
                                  reads=[q_t, ones_f])
                        pend.append(stats)
            while pend:
                pend.pop(0)()
            ln_finalize(ps1, ps2, chunks, O0, st1, mtmp, rtmp)
        fw.barrier()
        fw.pop()

    def ln_apply(z_t, mean_t, rstd_t, gi, bi, out_t):
        fw.op("dve", lambda e: e.tensor_tensor(out=z_t[:], in0=z_t[:], in1=mean_t[:, :].unsqueeze(1).broadcast_to([128, 32, 128]), op=ALU.subtract),
              reads=[z_t, mean_t], writes=[z_t])
        fw.op("dve", lambda e: e.tensor_tensor(out=z_t[:], in0=z_t[:], in1=rstd_t[:, :].unsqueeze(1).broadcast_to([128, 32, 128]), op=ALU.mult),
              reads=[z_t, rstd_t], writes=[z_t])
        for kc in range(32):
            fw.op("act", lambda e, kc=kc: e.activation(out=out_t[:, kc, :], in_=z_t[:, kc, :], func=AF.Identity,
                                                       scale=vc[:, gi, kc:kc + 1], bias=vc[:, bi, kc:kc + 1]), reads=[z_t, vc], writes=[out_t])

    if "p4" in phases:
        fw.new_phase("p4")
        fw.push()
        SB4 = 768
        xm2 = fw.sbuf("xm2", [128, 32, SB4], BF16)
        hid = fw.sbuf("hid", [128, 32, SB4], BF16)
        gT = fw.sbuf("gT", [16, SB4], F32)
        wr_s = fw.sbuf("wr_s", [128, 32, 20], F32, dma=True)
        br_s = fw.sbuf("br_s", [128, 20], F32, dma=True)
        sel_s = fw.sbuf("sel_s", [16, 16, 128], F32, dma=True)
        id_s = fw.sbuf("id_s", [128, 128], F32, dma=True)
        fw.dma("sp", wr_s, wr_s[:], w_r, w_r.ap.rearrange("(kc p) n -> p kc n", p=128))
        fw.dma("sp", br_s, br_s[:], b_r, b_r.ap.broadcast_to([128, 20]))
        fw.dma("sp", sel_s, sel_s[:], sel, sel.ap)
        fw.dma("sp", id_s, id_s[:], ident, ident.ap)
        for sb in range(3):
            O0 = sb * SB4
            chunks = [(0, 512, False), (512, 256, sb == 2)]
            fw.push()
            zb = fw.sbuf("zb", [128, 32, 128], F32, dma=True)
            h1b = fw.sbuf("h1b", [128, 32, 128], F32, dma=True)
            xf = fw.sbuf("xf", [128, 32, 128], F32)
            mt_ = fw.sbuf("mt_", [128, 128], F32, dma=True)
            rt_ = fw.sbuf("rt_", [128, 128], F32, dma=True)
            rs = fw.sbuf("rs", [128, 64], F32)
            gates = fw.sbuf("gates", [128, 16], F32)
            pR = fw.psum("pR", [128, 512])
            pT = fw.psum("pT", [128, 512])
            for tt in range(6):
                o = O0 + tt * 128
                isc = (o >= 2048)
                fw.dma("sp", zb, zb[:], z1T, z1T.ap[:, :, o:o + 128].rearrange("c p o -> p c o"))
                fw.dma("sp", mt_, mt_[:], st1, st1.ap[0][:, o:o + 128])
                fw.dma("sp", rt_, rt_[:], st1, st1.ap[1][:, o:o + 128])
                ln_apply(zb, mt_, rt_, 1, 2, h1b)
                fw.dma("sp", h1T, h1T.ap[:, :, o:o + 128].rearrange("c p o -> p c o"), h1b, h1b[:])
                mb = 6 if isc else 0
                fw.op("dve", lambda e, mb=mb: e.tensor_tensor(out=xf[:], in0=h1b[:], in1=mo[:, mb + 4, :].unsqueeze(2).broadcast_to([128, 32, 128]), op=ALU.mult),
                      reads=[h1b, mo], writes=[xf])
                fw.op("dve", lambda e, mb=mb: e.tensor_tensor(out=xf[:], in0=xf[:], in1=mo[:, mb + 3, :].unsqueeze(2).broadcast_to([128, 32, 128]), op=ALU.add),
                      reads=[xf, mo], writes=[xf])
                fw.op("act", lambda e, tt=tt: e.copy(out=xm2[:, :, tt * 128:(tt + 1) * 128], in_=xf[:]), reads=[xf], writes=[xm2])
                fw.mm(pR, [(lambda e, kc=kc: e.matmul(pR[:, 0:20], lhsT=xf[:, kc, :], rhs=wr_s[:, kc, :], start=(kc == 0), stop=(kc == 31)))
                           for kc in range(32)], reads=[xf, wr_s])
                LG, MX, NMX, EX, SM, GP, OH, LE, E3, EIN, M1, OH1, E2, M2, OH2, DF, W1, W2, WI = (
                    rs[:, 0:20], rs[:, 20:21], rs[:, 21:22], rs[:, 22:26], rs[:, 26:27], rs[:, 27:28], rs[:, 28:32], rs[:, 4:20],
                    rs[:, 32:48], rs[:, 48:52], rs[:, 52:53], rs[:, 53:57], rs[:, 57:61], rs[:, 61:62], rs[:, 22:26], rs[:, 62:63],
                    rs[:, 63:64], rs[:, 26:27], rs[:, 48:52])

                def V(fn, reads=(rs,), writes=(rs,)):
                    fw.op("dve", fn, reads=list(reads), writes=list(writes))
                V(lambda e: e.tensor_tensor(out=LG, in0=pR[:, 0:20], in1=br_s[:], op=ALU.add), reads=(pR, br_s))
                V(lambda e: e.reduce_max(out=MX, in_=rs[:, 0:4], axis=mybir.AxisListType.X))
                V(lambda e: e.tensor_scalar(out=OH, in0=rs[:, 0:4], scalar1=MX, scalar2=None, op0=ALU.is_equal))
                V(lambda e: e.tensor_scalar(out=NMX, in0=MX, scalar1=-1.0, scalar2=None, op0=ALU.mult))
                fw.op("act", lambda e: e.activation(out=EX, in_=rs[:, 0:4], func=AF.Exp, bias=NMX, scale=1.0), reads=[rs], writes=[rs])
                V(lambda e: e.reduce_sum(out=SM, in_=EX, axis=mybir.AxisListType.X))
                V(lambda e: e.reciprocal(out=GP, in_=SM))
                V(lambda e: e.tensor_tensor(out=E3.rearrange("p (g e) -> p g e", g=4), in0=LE.rearrange("p (g e) -> p g e", g=4),
                                            in1=OH.unsqueeze(2).broadcast_to([128, 4, 4]), op=ALU.mult))
                V(lambda e: e.tensor_tensor(out=EIN, in0=rs[:, 32:36], in1=rs[:, 36:40], op=ALU.add))
                V(lambda e: e.tensor_tensor(out=EIN, in0=EIN, in1=rs[:, 40:44], op=ALU.add))
                V(lambda e: e.tensor_tensor(out=EIN, in0=EIN, in1=rs[:, 44:48], op=ALU.add))
                V(lambda e: e.reduce_max(out=M1, in_=EIN, axis=mybir.AxisListType.X))
                V(lambda e: e.tensor_scalar(out=OH1, in0=EIN, scalar1=M1, scalar2=None, op0=ALU.is_equal))
                V(lambda e: e.scalar_tensor_tensor(out=E2, in0=OH1, scalar=-1e30, in1=EIN, op0=ALU.mult, op1=ALU.add))
                V(lambda e: e.reduce_max(out=M2, in_=E2, axis=mybir.AxisListType.X))
                V(lambda e: e.tensor_scalar(out=OH2, in0=E2, scalar1=M2, scalar2=None, op0=ALU.is_equal))
                V(lambda e: e.tensor_tensor(out=DF, in0=M2, in1=M1, op=ALU.subtract))
                fw.op("act", lambda e: e.activation(out=DF, in_=DF, func=AF.Exp), reads=[rs], writes=[rs])
                V(lambda e: e.tensor_scalar(out=W1, in0=DF, scalar1=1.0, scalar2=None, op0=ALU.add))
                V(lambda e: e.reciprocal(out=W1, in_=W1))
                V(lambda e: e.tensor_tensor(out=W2, in0=DF, in1=W1, op=ALU.mult))
                V(lambda e: e.tensor_tensor(out=W1, in0=W1, in1=GP, op=ALU.mult))
                V(lambda e: e.tensor_tensor(out=W2, in0=W2, in1=GP, op=ALU.mult))
                V(lambda e: e.tensor_scalar(out=WI, in0=OH1, scalar1=W1, scalar2=None, op0=ALU.mult))
                V(lambda e: e.scalar_tensor_tensor(out=WI, in0=OH2, scalar=W2, in1=WI, op0=ALU.mult, op1=ALU.add))
                fw.op("dve", lambda e: e.tensor_tensor(out=gates[:].rearrange("p (g e) -> p g e", g=4),
                                                       in0=OH.unsqueeze(2).broadcast_to([128, 4, 4]),
                                                       in1=WI.unsqueeze(1).broadcast_to([128, 4, 4]), op=ALU.mult), reads=[rs], writes=[gates])
                fw.mm(pT, [lambda e: e.transpose(out=pT[0:16, 0:128], in_=gates[:], identity=id_s[:])], reads=[gates, id_s])
                fw.op("act", lambda e, tt=tt: e.copy(out=gT[:, tt * 128:(tt + 1) * 128], in_=pT[0:16, 0:128]), reads=[pT], writes=[gT])
            fw.barrier()
            fw.pop()
            fw.push()
            wgu = [fw.sbuf(f"wgu{i}", [128, 32, 256], BF16, dma=True) for i in range(3)]
            gbc = [fw.sbuf(f"gbc{i}", [128, SB4], F32) for i in range(2)]
            sgt = [fw.sbuf(f"sgt{i}", [128, 512], F32) for i in range(2)]
            hres = [fw.sbuf(f"hres4{i}", [128, SB4], F32, dma=True) for i in range(2)]
            zt = [fw.sbuf(f"zt4{i}", [128, 512], F32, dma=True) for i in range(3)]
            zq = [fw.sbuf(f"zq4{i}", [128, 512], F32) for i in range(3)]
            mtmp = [fw.sbuf(f"mtmp4{i}", [128, 512], F32, dma=True) for i in range(2)]
            rtmp = [fw.sbuf(f"rtmp4{i}", [128, 512], F32, dma=True) for i in range(2)]
            pG = [fw.psum(f"pG{i}", [128, 512]) for i in range(2)]
            pU = [fw.psum(f"pU{i}", [128, 512]) for i in range(2)]
            ps1 = [fw.psum(f"p4s1{i}", [128, 512]) for i in range(2)]
            ps2 = [fw.psum(f"p4s2{i}", [128, 512]) for i in range(2)]
            cw = 0
            cg = 0
            for ex in range(16):
                wg_t = wgu[cw % 3]
                wu_t = wgu[(cw + 1) % 3]
                cw += 2
                fw.dma("pool", wg_t, wg_t[:], w_gate, w_gate.ap[ex].rearrange("(kc p) n -> p kc n", p=128))
                fw.dma("pool", wu_t, wu_t[:], w_up, w_up.ap[ex].rearrange("(kc p) n -> p kc n", p=128))
                g_t = gbc[ex % 2]
                for (q0, n, isc) in chunks:
                    Gp = pG[cg % 2]
                    fw.mm(Gp, [lambda e, Gp=Gp, q0=q0, n=n, ex=ex: e.matmul(Gp[:, 0:n], lhsT=sel_s[:, ex, :], rhs=gT[:, q0:q0 + n], start=True, stop=True)],
                          reads=[sel_s, gT])
                    fw.op("act", lambda e, Gp=Gp, g_t=g_t, q0=q0, n=n: e.copy(out=g_t[:, q0:q0 + n], in_=Gp[:, 0:n]), reads=[Gp], writes=[g_t])
                    cg += 1
                for fc in range(2):
                    for (q0, n, isc) in chunks:
                        Gp, Up = pG[cg % 2], pU[cg % 2]
                        s_t = sgt[cg % 2]
                        cg += 1
                        fw.mm(Gp, [(lambda e, kc=kc, Gp=Gp, fc=fc, q0=q0, n=n, wg_t=wg_t: e.matmul(
                            Gp[:, 0:n], lhsT=wg_t[:, kc, fc * 128:(fc + 1) * 128], rhs=xm2[:, kc, q0:q0 + n],
                            start=(kc == 0), stop=(kc == 31))) for kc in range(32)], reads=[xm2, wg_t])
                        fw.mm(Up, [(lambda e, kc=kc, Up=Up, fc=fc, q0=q0, n=n, wu_t=wu_t: e.matmul(
                            Up[:, 0:n], lhsT=wu_t[:, kc, fc * 128:(fc + 1) * 128], rhs=xm2[:, kc, q0:q0 + n],
                            start=(kc == 0), stop=(kc == 31))) for kc in range(32)], reads=[xm2, wu_t])
                        fw.op("act", lambda e, Gp=Gp, s_t=s_t, n=n: e.activation(out=s_t[:, 0:n], in_=Gp[:, 0:n], func=AF.Silu), reads=[Gp], writes=[s_t])
                        fw.op("dve", lambda e, Up=Up, s_t=s_t, n=n: e.tensor_tensor(out=s_t[:, 0:n], in0=Up[:, 0:n], in1=s_t[:, 0:n], op=ALU.mult),
                              reads=[Up, s_t], writes=[s_t])
                        fw.op("dve", lambda e, s_t=s_t, g_t=g_t, ex=ex, fc=fc, q0=q0, n=n: e.tensor_tensor(
                            out=hid[:, ex * 2 + fc, q0:q0 + n], in0=s_t[:, 0:n], in1=g_t[:, q0:q0 + n], op=ALU.mult),
                            reads=[s_t, g_t], writes=[hid])
            pend = []
            ca = 0
            cz = 0
            for ct in range(16):
                w_t = wgu[cw % 3]
                cw += 1
                fw.dma("pool", w_t, w_t[:], w_down, w_down.ap[:, ct * 256:(ct + 1) * 256].rearrange("(kc p) n -> p kc n", p=128))
                for sub in range(2):
                    dc = ct * 2 + sub
                    h_t = hres[dc % 2]
                    fw.dma("sp", h_t, h_t[:], h1T, h1T.ap[dc][:, O0:O0 + SB4])
                    for ci, (q0, n, isc) in enumerate(chunks):
                        A = pG[ca % 2] if (ca // 2) % 2 == 0 else pU[ca % 2]
                        ca += 1
                        fw.mm(A, [(lambda e, kc=kc, A=A, sub=sub, q0=q0, n=n, w_t=w_t: e.matmul(
                            A[:, 0:n], lhsT=w_t[:, kc, sub * 128:(sub + 1) * 128], rhs=hid[:, kc, q0:q0 + n],
                            start=(kc == 0), stop=(kc == 31))) for kc in range(32)], reads=[hid, w_t])
                        while pend:
                            pend.pop(0)()
                        z_t, q_t = zt[cz % 3], zq[cz % 3]
                        cz += 1
                        fw.op("dve", lambda e, z_t=z_t, A=A, dc=dc, isc=isc, h_t=h_t, q0=q0, n=n: e.scalar_tensor_tensor(
                            out=z_t[:, 0:n], in0=A[:, 0:n], scalar=MO(5, isc, dc), in1=h_t[:, q0:q0 + n], op0=ALU.mult, op1=ALU.add),
                            reads=[A, h_t, mo], writes=[z_t])
                        fw.op("act", lambda e, z_t=z_t, q_t=q_t, n=n: e.activation(out=q_t[:, 0:n], in_=z_t[:, 0:n], func=AF.Square),
                              reads=[z_t], writes=[q_t])
                        fw.dma("sp", z2T, z2T.ap[dc][:, O0 + q0:O0 + q0 + n], z_t, z_t[:, 0:n])

                        def stats(z_t=z_t, q_t=q_t, ci=ci, n=n, dc=dc):
                            fw.mm(ps1[ci], [lambda e: e.matmul(ps1[ci][:, 0:n], lhsT=ones_f[:], rhs=z_t[:, 0:n], start=(dc == 0), stop=(dc == 31))],
                                  reads=[z_t, ones_f])
                            fw.mm(ps2[ci], [lambda e: e.matmul(ps2[ci][:, 0:n], lhsT=ones_f[:], rhs=q_t[:, 0:n], start=(dc == 0), stop=(dc == 31))],
                                  reads=[q_t, ones_f])
                        pend.append(stats)
            while pend:
                pend.pop(0)()
            ln_finalize(ps1, ps2, chunks, O0, st2, mtmp, rtmp)
            fw.barrier()
            fw.pop()
        fw.barrier()
        fw.pop()

    if "p5" in phases:
        fw.new_phase("p5")
        fw.push()
        zb = [fw.sbuf(f"zb5{i}", [128, 32, 128], F32, dma=True) for i in range(2)]
        ob = [fw.sbuf(f"ob5{i}", [128, 32, 128], F32, dma=True) for i in range(2)]
        mt_ = [fw.sbuf(f"mt5{i}", [128, 128], F32, dma=True) for i in range(2)]
        rt_ = [fw.sbuf(f"rt5{i}", [128, 128], F32, dma=True) for i in range(2)]
        for tt in range(18):
            o = tt * 128
            z_t, o_t, m_t, r_t = zb[tt % 2], ob[tt % 2], mt_[tt % 2], rt_[tt % 2]
            fw.dma("sp", z_t, z_t[:], z2T, z2T.ap[:, :, o:o + 128].rearrange("c p o -> p c o"))
            fw.dma("sp", m_t, m_t[:], st2, st2.ap[0][:, o:o + 128])
            fw.dma("sp", r_t, r_t[:], st2, st2.ap[1][:, o:o + 128])
            ln_apply(z_t, m_t, r_t, 3, 4, o_t)
            fw.dma("sp", hout, hout.ap[:, :, o:o + 128].rearrange("c p o -> p c o"), o_t, o_t[:])
        fw.barrier()
        fw.pop()

    fw.barrier()
    fw.wait("sp", hout.w)
    fw.close()
    return nc, fw


def attention_block_swa(fw, rhsQ, keys, K_t, V_t, Q_t, pS, pO, pDn, Pb, cnt, kvb_s, ones_b, finalize):
    O = pO[cnt["o"] % 2]
    Dn = pDn[cnt["o"] % 2]
    cnt["o"] += 1
    pend = []
    nk = len(keys)
    r4 = lambda ap: ap.rearrange("p (h q) -> p h q", h=4)
    for idx, (kt, mlist) in enumerate(keys):
        S_ = pS[cnt["s"] % len(pS)]
        P_ = Pb[cnt["s"] % len(Pb)]
        cnt["s"] += 1
        fw.mm(S_, [lambda e, S_=S_, kt=kt: e.matmul(r4(S_[:]), lhsT=K_t[:, kt * 128:(kt + 1) * 128], rhs=rhsQ, start=True, stop=True)],
              reads=[K_t, Q_t])
        while pend:
            pend.pop(0)()
        fw.op("act", lambda e, S_=S_, P_=P_, kt=kt: e.activation(out=P_[:], in_=S_[:], func=AF.Exp, scale=SCL, bias=kvb_s[:, kt:kt + 1]),
              reads=[S_, kvb_s], writes=[P_])
        for (mt, map_) in mlist:
            fw.op("dve", lambda e, P_=P_, map_=map_: e.tensor_tensor(out=P_[:], in0=P_[:], in1=map_, op=ALU.mult), reads=[P_, mt], writes=[P_])

        def pv(P_=P_, kt=kt, idx=idx):
            fw.mm(O, [lambda e: e.matmul(O[:], lhsT=V_t[:, kt, :], rhs=P_[:], start=(idx == 0), stop=(idx == nk - 1))], reads=[V_t, P_])
            fw.mm(Dn, [lambda e: e.matmul(Dn[:], lhsT=ones_b[:], rhs=P_[:], start=(idx == 0), stop=(idx == nk - 1))], reads=[ones_b, P_])
        pend.append(pv)
    while pend:
        pend.pop(0)()
    finalize(O, Dn)


def fm(vec):
    v = np.asarray(vec, np.float32)
    lead = v.shape[:-1]
    v = v.reshape(lead + (32, 128))
    return np.ascontiguousarray(np.moveaxis(v, -1, 0))


def const_tables(half):
    s0 = half * 2048
    t = np.arange(EXT)
    s = s0 - HALO + t
    valid = (s >= 0) & (s < S)
    sc = np.clip(s, 0, S - 1)
    pos = np.stack([sc // 64, sc % 64], -1).astype(np.float32)
    inv_freq = (np.float32(10000.0) ** (-np.arange(32, dtype=np.float32) / np.float32(32))).astype(np.float32)
    ang = (pos[:, :, None] * inv_freq).astype(np.float32)
    cs, sn = np.cos(ang).astype(np.float32), np.sin(ang).astype(np.float32)
    d = np.arange(128)
    axis, hf, f = d // 64, (d % 64) // 32, d % 32
    rope = np.zeros((2, 128, TOK), np.float32)
    rope[0, :, :EXT] = cs[:, axis, f].T
    rope[1, :, :EXT] = (sn[:, axis, f] * np.where(hf == 0, -1.0, 1.0)[None, :]).T
    rope[0, :, EXT:] = 1.0
    partner = np.where(hf == 0, d + 32, d - 32)
    perm = np.zeros((128, 128), np.float32)
    perm[partner, d] = 1.0
    kvb = np.zeros((128, 22), np.float32)
    kvb[:, :20] = np.where(valid, 0.0, NEGB).reshape(20, 128).T
    cedge = np.zeros((128, 2), np.float32)
    cedge[:, 0] = 1.0 if s0 - 1 >= 0 else 0.0
    cedge[:, 1] = 1.0 if s0 + 2048 < S else 0.0
    k = np.arange(128)[:, None]
    q = np.arange(128)[None, :]
    swam = np.zeros((128, 2, 4, 128), np.float32)
    swam[:, 0] = (k >= q)[:, None, :]
    swam[:, 1] = (k <= q)[:, None, :]
    swam = swam.reshape(128, 2, 512)
    R0 = half * 32
    natm = np.zeros((128, 4, 8, 512), np.float32)
    kk = np.arange(128)
    qq = np.arange(512)
    for i in range(4):
        for j in range(8):
            kr = R0 + 8 * i - 4 + 2 * j + kk // 64
            kc = kk % 64
            qr = R0 + 8 * i + qq // 64
            qc = qq % 64
            rs = np.clip(qr - 4, 0, 56)
            rowok = (kr[:, None] >= rs[None, :]) & (kr[:, None] <= rs[None, :] + 7) & (kr[:, None] >= 0) & (kr[:, None] < 64)
            cst = np.clip(qc - 8, 0, 48)
            colok = (kc[:, None] >= cst[None, :]) & (kc[:, None] < cst[None, :] + 16)
            natm[:, i, j, :] = rowok & colok
    sel = np.zeros((16, 16, 128), np.float32)
    for e in range(16):
        sel[e, e, :] = 1.0
    return dict(rope=rope, perm=perm.astype(ml_dtypes.bfloat16), kvb=kvb, cedge=cedge, swam=swam.astype(ml_dtypes.bfloat16),
                natm=natm.astype(ml_dtypes.bfloat16), sel=sel, ident=np.eye(128, dtype=np.float32))


def nat_bias_table(rpb):
    kk = np.arange(128)
    qq = np.arange(512)
    out = np.zeros((8, 128, 8, 512), np.float32)
    for j in range(8):
        dr = (-4 + 2 * j + kk // 64)[:, None] - (qq // 64)[None, :] + 7
        dc = (kk % 64)[:, None] - (qq % 64)[None, :] + 15
        out[:, :, j, :] = rpb[:, np.clip(dr, 0, 14), np.clip(dc, 0, 30)]
    return out


_CACHE = {}


def _get(name, fn):
    if name not in _CACHE:
        _CACHE[name] = fn()
    return _CACHE[name]


def run_ada(c, c_ctx, w_ada, b_ada):
    nc = _get("ada", build_ada)
    cc = np.zeros((8, D), np.float32)
    cc[:4] = c
    cc[4] = c_ctx
    cT = np.ascontiguousarray(cc.reshape(8, 32, 128).transpose(2, 1, 0))
    in_maps = [{"cT": cT, "wa": np.ascontiguousarray(w_ada[:, :, k * ADA_CS:(k + 1) * ADA_CS]),
                "ba": np.ascontiguousarray(b_ada[:, k * ADA_CS:(k + 1) * ADA_CS])} for k in range(8)]
    res = run_bass_kernel_spmd(nc, in_maps, core_ids=list(range(8)))
    return np.concatenate([r["mod"] for r in res.results], axis=2)


def layer_inputs(l, core, h, hc, mod, P, tabs):
    b, half = core // 2, core % 2
    s0 = half * 2048
    ext = np.zeros((TOK, D), np.float32)
    lo, hi = s0 - HALO, s0 + 2048 + HALO
    a, e = max(lo, 0), min(hi, S)
    ext[a - lo:e - lo] = h[b, a:e]
    ext[EXT:] = hc[b]
    hT = np.ascontiguousarray(ext.T.reshape(32, 128, TOK))
    mv = np.concatenate([fm(mod[l, b].reshape(6, D)), fm(mod[l, 4].reshape(6, D))], axis=1)
    t = tabs[half]
    d = dict(hT=hT, modv=np.ascontiguousarray(mv))
    d.update(P)
    d.update(t)
    return d


def layer_params(l, inp):
    P = {}
    P["w_in"] = np.ascontiguousarray(inp["w_in"][l])
    P["w_out"] = np.ascontiguousarray(inp["w_out"][l])
    P["w_gate"] = np.ascontiguousarray(inp["w_gate"][l])
    P["w_up"] = np.ascontiguousarray(inp["w_up"][l])
    P["w_down"] = np.ascontiguousarray(inp["w_down"][l].reshape(D, D))
    P["w_r"] = np.ascontiguousarray(np.concatenate([inp["w_router_group"][l], inp["w_router_expert"][l]], axis=1))
    P["b_r"] = np.ascontiguousarray(np.concatenate([inp["b_router_group"][l], inp["b_router_expert"][l]])[None, :])
    cw = np.concatenate([inp["conv_w"][l], inp["conv_b"][l][None, :]], axis=0)
    P["convp"] = np.ascontiguousarray(cw.reshape(4, 8, 128).transpose(2, 1, 0))
    P["sink"] = np.ascontiguousarray(inp["attn_sink"][l][None, :])
    P["vecs"] = fm(np.stack([inp["mix_norm_g"][l], inp["ln1_g"][l], inp["ln1_b"][l], inp["ln2_g"][l], inp["ln2_b"][l]]))
    P["natb"] = nat_bias_table(np.asarray(inp["nat_rpb"][l]))
    return P


def run_layer(l, h, hc, mod, inp, tabs, cores=range(8)):
    nc, _ = _get("layer", build_layer)
    P = layer_params(l, inp)
    in_maps = [layer_inputs(l, c, h, hc, mod, P, tabs) for c in cores]
    res = run_bass_kernel_spmd(nc, in_maps, core_ids=list(range(len(in_maps))))
    return [r["hout"] for r in res.results]


def kernel(**inp):
    inp = {k: np.asarray(v) for k, v in inp.items()}
    mod = run_ada(inp["c"], inp["c_ctx"], inp["w_ada"], inp["b_ada"])
    tabs = [const_tables(0), const_tables(1)]
    h = np.array(inp["x"], np.float32, copy=True)
    hc = np.array(inp["ctx"], np.float32, copy=True)
    for l in range(NL):
        outs = run_layer(l, h, hc, mod, inp, tabs)
        for c in range(8):
            b, half = c // 2, c % 2
            o = outs[c].reshape(D, OWN).T
            h[b, half * 2048:(half + 1) * 2048] = o[:2048]
            if half == 0:
                hc[b] = o[2048:]
    return h
```

```python
import numpy as np
import ml_dtypes
import concourse.bass as bass
import concourse.mybir as mybir
from concourse.bass_utils import run_bass_kernel_spmd

F32 = mybir.dt.float32
BF16 = mybir.dt.bfloat16
AF = mybir.ActivationFunctionType
ALU = mybir.AluOpType

D = 4096
NL = 4
S = 4096
B = 4
CTX = 256
IN_COLS = 9216
ALPHA = (2.0 * NL) ** 0.25
LN_EPS_P = 1e-5 / (ALPHA * ALPHA)
RMS_EPS = 1e-6
SCL = 128.0 ** -0.5
NEGB = -30000.0

EXT = 2560
TOK = EXT + CTX
OWN = 2048 + CTX
HALO = 256


class Tile:
    def __init__(self, ap, name="", dsem=None):
        self.ap = ap
        self.name = name
        self.w = {}
        self.r = {}
        self.dsem = dsem
        self.excl = False

    def __getitem__(self, k):
        return self.ap[k]


class FW:
    def __init__(self, nc):
        self.nc = nc
        self.eng = {"pe": nc.tensor, "act": nc.scalar, "dve": nc.vector, "pool": nc.gpsimd, "sp": nc.sync}
        self.prog = {}
        self.waited = {k: {} for k in self.eng}
        self._ctx = []
        self._persist = []
        self._scopes = []
        self.allsems = []
        self.n_ins = 0
        self.uid = 0
        self.free_dsems = []
        self._scope_dsems = [[]]

    def sem(self, name):
        return self.sem_persist(name)

    def new_phase(self, tag):
        for e in ("pe", "act", "dve", "pool"):
            self.prog[e] = self.sem(f"p_{tag}_{e}")

    def push(self):
        self._scopes.append(len(self._ctx))
        self._scope_dsems.append([])

    def pop(self):
        n = self._scopes.pop()
        while len(self._ctx) > n:
            cm = self._ctx.pop()
            cm.__exit__(None, None, None)
        self.free_dsems.extend(self._scope_dsems.pop())

    def dsem_get(self):
        if self.free_dsems:
            rec = self.free_dsems.pop()
        else:
            rec = self.sem_persist(f"dq{len(self.allsems)}")
        self._scope_dsems[-1].append(rec)
        return rec

    def sem_persist(self, name):
        cm = self.nc.semaphore(name)
        h = cm.__enter__()
        self._persist.append(cm)
        rec = [h, 0, name]
        self.allsems.append(rec)
        return rec

    def sbuf(self, name, shape, dtype, dma=False):
        self.uid += 1
        name = f"{name}_{self.uid}"
        cm = self.nc.sbuf_tensor(name, shape, dtype)
        h = cm.__enter__()
        self._ctx.append(cm)
        ds = self.dsem_get() if dma else None
        return Tile(h, name, ds)

    def psum(self, name, shape, dtype=F32):
        self.uid += 1
        name = f"{name}_{self.uid}"
        cm = self.nc.psum_tensor(name, shape, dtype)
        h = cm.__enter__()
        self._ctx.append(cm)
        t = Tile(h, name)
        t.excl = True
        return t

    def dram(self, name, shape, dtype, kind="Internal"):
        h = self.nc.dram_tensor(name, shape, dtype, kind=kind)
        return Tile(h.ap(), name)

    def close(self):
        while self._ctx:
            self._ctx.pop().__exit__(None, None, None)
        while self._persist:
            self._persist.pop().__exit__(None, None, None)

    def wait(self, e, toks, skip_same=None):
        wd = self.waited[e]
        skip_nm = None
        if skip_same is not None and skip_same in self.prog:
            skip_nm = self.prog[skip_same][2]
        for nm, (s, v) in list(toks.items()):
            if nm == skip_nm:
                continue
            if wd.get(nm, 0) >= v:
                continue
            self.eng[e].wait_ge(s, v)
            wd[nm] = v

    @staticmethod
    def _merge(d, rec, v):
        nm = rec[2]
        if nm not in d or d[nm][1] < v:
            d[nm] = (rec[0], v)

    def op(self, e, emit, reads=(), writes=()):
        for t in reads:
            self.wait(e, t.w)
            if t.excl:
                self.wait(e, t.r, skip_same=e)
        for t in writes:
            self.wait(e, t.w, skip_same=e)
            self.wait(e, t.r, skip_same=e)
        ins = emit(self.eng[e])
        p = self.prog[e]
        p[1] += 1
        ins.then_inc(p[0], 1)
        self.n_ins += 1
        for t in reads:
            self._merge(t.r, p, p[1])
        for t in writes:
            self._merge(t.w, p, p[1])
        return ins

    def mm(self, out_t, emit_list, reads=()):
        e = "pe"
        for t in reads:
            self.wait(e, t.w)
        self.wait(e, out_t.w, skip_same=e)
        self.wait(e, out_t.r, skip_same=e)
        ins = None
        for f in emit_list:
            ins = f(self.eng[e])
            self.n_ins += 1
        p = self.prog[e]
        p[1] += 1
        ins.then_inc(p[0], 1)
        for t in reads:
            self._merge(t.r, p, p[1])
        self._merge(out_t.w, p, p[1])

    def dma(self, q, out_t, out_ap, in_t, in_ap, semtile=None):
        st = semtile if semtile is not None else (out_t if out_t.dsem is not None else in_t)
        ds = st.dsem
        assert ds is not None, (out_t.name, in_t.name)
        self.wait(q, in_t.w)
        self.wait(q, out_t.w)
        self.wait(q, out_t.r)
        ins = self.eng[q].dma_start(out=out_ap, in_=in_ap)
        ds[1] += 16
        ins.then_inc(ds[0], 16)
        self.n_ins += 1
        self._merge(in_t.r, ds, ds[1])
        self._merge(out_t.w, ds, ds[1])
        return ins

    def barrier(self):
        toks = {}
        for rec in self.allsems:
            if rec[1] > 0:
                toks[rec[2]] = (rec[0], rec[1])
        for e in ("sp", "pool", "act", "dve", "pe"):
            self.wait(e, toks)


def _f(fn, *a, **k):
    return lambda eng: fn(eng, *a, **k)


ADA_CS = 3072


def build_ada():
    nc = bass.Bass("TRN2", target_bir_lowering=False)
    fw = FW(nc)
    fw.new_phase("a")
    cT = fw.dram("cT", [128, 32, 8], F32, kind="ExternalInput")
    wa = fw.dram("wa", [NL, D, ADA_CS], F32, kind="ExternalInput")
    ba = fw.dram("ba", [NL, ADA_CS], F32, kind="ExternalInput")
    out = fw.dram("mod", [NL, 8, ADA_CS], F32, kind="ExternalOutput")
    CW = 256
    nct = ADA_CS // CW
    c_sb = fw.sbuf("c_sb", [128, 32, 8], F32, dma=True)
    sg = fw.sbuf("sg", [128, 32, 8], F32)
    act = fw.sbuf("actv", [128, 32, 8], F32)
    wr = [fw.sbuf(f"w{i}", [128, 32, CW], F32, dma=True) for i in range(2)]
    bb = [fw.sbuf(f"bb{i}", [8, ADA_CS], F32, dma=True) for i in range(2)]
    res = [fw.sbuf(f"res{i}", [8, ADA_CS], F32, dma=True) for i in range(2)]
    ps = [fw.psum(f"ps{i}", [8, 512]) for i in range(2)]
    fw.dma("sp", c_sb, c_sb[:], cT, cT.ap)
    fw.op("act", lambda e: e.activation(out=sg[:], in_=c_sb[:], func=AF.Sigmoid), reads=[c_sb], writes=[sg])
    fw.op("dve", lambda e: e.tensor_tensor(out=act[:], in0=sg[:], in1=c_sb[:], op=ALU.mult), reads=[sg, c_sb], writes=[act])
    i = 0
    for l in range(NL):
        b_t = bb[l % 2]
        r_t = res[l % 2]
        fw.dma("sp", b_t, b_t[:], ba, ba.ap[l:l + 1, :].broadcast_to([8, ADA_CS]))
        for ct in range(nct):
            w_t = wr[i % 2]
            p_t = ps[i % 2]
            fw.dma("sp", w_t, w_t[:], wa, wa.ap[l, :, ct * CW:(ct + 1) * CW].rearrange("(kc p) n -> p kc n", p=128))
            fw.mm(p_t, [(lambda e, kc=kc, w_t=w_t, p_t=p_t: e.matmul(p_t[:, 0:CW], lhsT=act[:, kc, :], rhs=w_t[:, kc, :],
                                                                    start=(kc == 0), stop=(kc == 31))) for kc in range(32)],
                  reads=[act, w_t])
            fw.op("dve", lambda e, p_t=p_t, r_t=r_t, b_t=b_t, ct=ct: e.tensor_tensor(
                out=r_t[:, ct * CW:(ct + 1) * CW], in0=p_t[:, 0:CW], in1=b_t[:, ct * CW:(ct + 1) * CW], op=ALU.add),
                reads=[p_t, b_t], writes=[r_t])
            i += 1
        fw.dma("sp", out, out.ap[l], r_t, r_t[:])
    fw.wait("sp", out.w)
    fw.close()
    return nc


def own_segments(t0, n):
    segs = []
    a, b = max(t0, HALO), min(t0 + n, HALO + 2048)
    if a < b:
        segs.append((a, b - a, a - HALO))
    a, b = max(t0, EXT), min(t0 + n, TOK)
    if a < b:
        segs.append((a, b - a, a - EXT + 2048))
    return segs


def own_to_tok(o0, n):
    segs = []
    a, b = max(o0, 0), min(o0 + n, 2048)
    if a < b:
        segs.append((a, b - a, a + HALO))
    a, b = max(o0, 2048), min(o0 + n, OWN)
    if a < b:
        segs.append((a, b - a, a - 2048 + EXT))
    return segs


def build_layer(phases=("p1", "p2a", "p2b", "p2c", "p3", "p4", "p5"), dbg=False):
    nc = bass.Bass("TRN2", target_bir_lowering=False)
    fw = FW(nc)
    EI = "ExternalInput"
    hT = fw.dram("hT", [32, 128, TOK], F32, kind=EI)
    modv = fw.dram("modv", [128, 12, 32], F32, kind=EI)
    w_in = fw.dram("w_in", [D, IN_COLS], F32, kind=EI)
    w_out = fw.dram("w_out", [D, D], F32, kind=EI)
    w_gate = fw.dram("w_gate", [16, D, 256], F32, kind=EI)
    w_up = fw.dram("w_up", [16, D, 256], F32, kind=EI)
    w_down = fw.dram("w_down", [D, D], F32, kind=EI)
    w_r = fw.dram("w_r", [D, 20], F32, kind=EI)
    b_r = fw.dram("b_r", [1, 20], F32, kind=EI)
    convp = fw.dram("convp", [128, 8, 4], F32, kind=EI)
    sink = fw.dram("sink", [1, 16], F32, kind=EI)
    vecs = fw.dram("vecs", [128, 5, 32], F32, kind=EI)
    rope = fw.dram("rope", [2, 128, TOK], F32, kind=EI)
    perm = fw.dram("perm", [128, 128], BF16, kind=EI)
    ident = fw.dram("ident", [128, 128], F32, kind=EI)
    natb = fw.dram("natb", [8, 128, 8, 512], F32, kind=EI)
    natm = fw.dram("natm", [128, 4, 8, 512], BF16, kind=EI)
    kvb = fw.dram("kvb", [128, 22], F32, kind=EI)
    cedge = fw.dram("cedge", [128, 2], F32, kind=EI)
    swam = fw.dram("swam", [128, 2, 512], BF16, kind=EI)
    sel = fw.dram("sel", [16, 16, 128], F32, kind=EI)
    hout = fw.dram("hout", [32, 128, OWN], F32, kind="ExternalOutput")

    QsT = fw.dram("QsT", [16, 128, OWN], BF16)
    KsT = fw.dram("KsT", [4, 128, TOK], BF16)
    Vs = fw.dram("Vs", [22, 128, 512], BF16)
    QnT = fw.dram("QnT", [8, 128, OWN], BF16)
    KnT = fw.dram("KnT", [8, 128, TOK], BF16)
    Vn = fw.dram("Vn", [22, 128, 1024], BF16)
    cvT = fw.dram("cvT", [24, 128, TOK], F32)
    yT = fw.dram("yT", [32, 128, OWN], BF16)
    z1T = fw.dram("z1T", [32, 128, OWN], F32)
    h1T = fw.dram("h1T", [32, 128, OWN], F32)
    z2T = fw.dram("z2T", [32, 128, OWN], F32)
    st1 = fw.dram("st1", [2, 128, OWN], F32)
    st2 = fw.dram("st2", [2, 128, OWN], F32)
    dbg_t = {}

    fw.new_phase("c")
    mo = fw.sbuf("mo", [128, 12, 32], F32, dma=True)
    vc = fw.sbuf("vc", [128, 5, 32], F32, dma=True)
    kvb_s = fw.sbuf("kvb_s", [128, 22], F32, dma=True)
    ones_b = fw.sbuf("ones_b", [128, 128], BF16)
    ones_f = fw.sbuf("ones_f", [128, 128], F32)
    fw.dma("sp", mo, mo[:], modv, modv.ap)
    fw.dma("sp", vc, vc[:], vecs, vecs.ap)
    fw.dma("sp", kvb_s, kvb_s[:], kvb, kvb.ap)
    fw.op("dve", lambda e: e.memset(ones_b[:], 1.0), writes=[ones_b])
    fw.op("dve", lambda e: e.memset(ones_f[:], 1.0), writes=[ones_f])
    for m in (1, 4, 7, 10):
        fw.op("dve", lambda e, m=m: e.tensor_scalar(out=mo[:, m, :], in0=mo[:, m, :], scalar1=1.0, scalar2=None, op0=ALU.add),
              reads=[mo], writes=[mo])
    for m in (2, 5, 8, 11):
        fw.op("dve", lambda e, m=m: e.tensor_scalar(out=mo[:, m, :], in0=mo[:, m, :], scalar1=1.0 / ALPHA, scalar2=None, op0=ALU.mult),
              reads=[mo], writes=[mo])
    fw.barrier()

    def MO(m, ctx, kc):
        mm_ = m + (6 if ctx else 0)
        return mo[:, mm_, kc:kc + 1]

    if "p1" in phases:
        fw.new_phase("p1")
        fw.push()
        SBT = 1408
        WT = 256
        xm = fw.sbuf("xm", [128, 32, SBT], BF16)
        hst = [fw.sbuf(f"hst{i}", [128, 2, SBT], F32, dma=True) for i in range(2)]
        wt = [fw.sbuf(f"wt{i}", [128, 32, WT], BF16, dma=True) for i in range(3)]
        rp = fw.sbuf("rp", [128, 2, SBT], F32, dma=True)
        pm = fw.sbuf("pm", [128, 128], BF16, dma=True)
        xb = [fw.sbuf(f"xb{i}", [128, 512], BF16) for i in range(2)]
        t1 = [fw.sbuf(f"t1{i}", [128, 512], F32) for i in range(2)]
        t2 = [fw.sbuf(f"t2{i}", [128, 512], F32) for i in range(2)]
        s16 = [fw.sbuf(f"s16{i}", [128, 512], BF16, dma=True) for i in range(4)]
        s32 = [fw.sbuf(f"s32{i}", [128, 512], F32, dma=True) for i in range(3)]
        pA = [fw.psum(f"pA{i}", [128, 512]) for i in range(4)]
        pB = [fw.psum(f"pB{i}", [128, 512]) for i in range(2)]
        fw.dma("sp", pm, pm[:], perm, perm.ap)
        cnt = {"a": 0, "b": 0, "s16": 0, "s32": 0, "r": 0, "w": 0, "h": 0}
        deferred = []

        def run_deferred():
            while deferred:
                deferred.pop(0)()

        wtiles = []
        for i in range(8):
            wtiles.append((i * WT, "ropeq", i))
        for i in range(2):
            wtiles.append((2048 + i * WT, "ropek", i))
        for i in range(2):
            wtiles.append((2560 + i * WT, "vs", i))
        for i in range(12):
            wtiles.append((3072 + i * WT, "conv", i))
        for i in range(4):
            wtiles.append((6144 + i * WT, "natq", i))
        for i in range(4):
            wtiles.append((7168 + i * WT, "natk", i))
        for i in range(4):
            wtiles.append((8192 + i * WT, "vn", i))

        import os as _os
        _kinds = _os.environ.get("P1_KINDS")
        if _kinds:
            wtiles = [w for w in wtiles if w[1] in _kinds.split(",")]
        _nw = int(_os.environ.get("P1_NW", "0"))
        if _nw:
            wtiles = wtiles[:_nw]
        for sb in range(int(_os.environ.get("P1_SB", "2"))):
            T0 = sb * SBT
            fw.dma("sp", rp, rp[:], rope, rope.ap[:, :, T0:T0 + SBT].rearrange("c p t -> p c t"))
            for pc in range(16):
                h_t = hst[cnt["h"] % 2]
                cnt["h"] += 1
                fw.dma("sp", h_t, h_t[:], hT, hT.ap[pc * 2:(pc + 1) * 2, :, T0:T0 + SBT].rearrange("kc p t -> p kc t"))
                for kk in range(2):
                    kc = pc * 2 + kk
                    parts = [(0, SBT, False)] if sb == 0 else [(0, EXT - T0, False), (EXT - T0, SBT, True)]
                    for (a, b_, isc) in parts:
                        if kc % 2 == 0:
                            fw.op("act", lambda e, h_t=h_t, kk=kk, kc=kc, a=a, b_=b_, isc=isc: e.activation(
                                out=xm[:, kc, a:b_], in_=h_t[:, kk, a:b_], func=AF.Identity,
                                scale=MO(1, isc, kc), bias=MO(0, isc, kc)), reads=[h_t, mo], writes=[xm])
                        else:
                            fw.op("dve", lambda e, h_t=h_t, kk=kk, kc=kc, a=a, b_=b_, isc=isc: e.tensor_scalar(
                                out=xm[:, kc, a:b_], in0=h_t[:, kk, a:b_], scalar1=MO(1, isc, kc), scalar2=MO(0, isc, kc),
                                op0=ALU.mult, op1=ALU.add), reads=[h_t, mo], writes=[xm])
            chunks = [(0, 512), (512, 512), (1024, 384)]
            for (c0, kind, info) in wtiles:
                w_t = wt[cnt["w"] % 3]
                cnt["w"] += 1
                fw.dma("pool", w_t, w_t[:], w_in, w_in.ap[:, c0:c0 + WT].rearrange("(kc p) n -> p kc n", p=128))
                if kind in ("vs", "vn"):
                    for tt in range(11):
                        A = pA[cnt["a"] % 4]
                        cnt["a"] += 1
                        fw.mm(A, [(lambda e, kc=kc, A=A, tt=tt, w_t=w_t: e.matmul(
                            A[:, 0:WT], lhsT=xm[:, kc, tt * 128:(tt + 1) * 128], rhs=w_t[:, kc, :],
                            start=(kc == 0), stop=(kc == 31))) for kc in range(32)], reads=[xm, w_t])
                        run_deferred()
                        st = s16[cnt["s16"] % 4]
                        cnt["s16"] += 1
                        if tt % 2 == 0:
                            fw.op("act", lambda e, st=st, A=A: e.copy(out=st[:, 0:WT], in_=A[:, 0:WT]), reads=[A], writes=[st])
                        else:
                            fw.op("dve", lambda e, st=st, A=A: e.tensor_copy(out=st[:, 0:WT], in_=A[:, 0:WT]), reads=[A], writes=[st])
                        gt = sb * 11 + tt
                        if kind == "vs":
                            fw.dma("sp", Vs, Vs.ap[gt][:, info * WT:(info + 1) * WT], st, st[:, 0:WT])
                        else:
                            fw.dma("sp", Vn, Vn.ap[gt][:, info * WT:(info + 1) * WT], st, st[:, 0:WT])
                    continue
                for sub in range(WT // 128):
                    for (q0, n) in chunks:
                        A = pA[cnt["a"] % 4]
                        cnt["a"] += 1
                        fw.mm(A, [(lambda e, kc=kc, A=A, sub=sub, q0=q0, n=n, w_t=w_t: e.matmul(
                            A[:, 0:n], lhsT=w_t[:, kc, sub * 128:(sub + 1) * 128], rhs=xm[:, kc, q0:q0 + n],
                            start=(kc == 0), stop=(kc == 31))) for kc in range(32)], reads=[xm, w_t])
                        run_deferred()
                        tg = T0 + q0
                        idx2 = info * (WT // 128) + sub
                        if kind in ("ropeq", "ropek"):
                            r = cnt["r"] % 2
                            cnt["r"] += 1
                            xb_t, t1_t, t2_t = xb[r], t1[r], t2[r]
                            Bp = pB[r]
                            fw.op("act", lambda e, xb_t=xb_t, A=A, n=n: e.copy(out=xb_t[:, 0:n], in_=A[:, 0:n]),
                                  reads=[A], writes=[xb_t])
                            fw.op("dve", lambda e, t1_t=t1_t, A=A, n=n, q0=q0: e.tensor_tensor(
                                out=t1_t[:, 0:n], in0=A[:, 0:n], in1=rp[:, 0, q0:q0 + n], op=ALU.mult),
                                reads=[A, rp], writes=[t1_t])

                            def part2(xb_t=xb_t, t1_t=t1_t, t2_t=t2_t, Bp=Bp, n=n, tg=tg, q0=q0, kind=kind, idx2=idx2):
                                _stg = int(_os.environ.get("ROPE_STAGE", "3"))
                                if _stg >= 2:
                                    fw.mm(Bp, [lambda e: e.matmul(Bp[:, 0:n], lhsT=pm[:], rhs=xb_t[:, 0:n], start=True, stop=True)],
                                          reads=[pm, xb_t])
                                if _stg >= 3:
                                    fw.op("dve", lambda e: e.tensor_tensor(out=t2_t[:, 0:n], in0=Bp[:, 0:n], in1=rp[:, 1, q0:q0 + n],
                                                                           op=ALU.mult), reads=[Bp, rp], writes=[t2_t])
                                st = s16[cnt["s16"] % 4]
                                cnt["s16"] += 1
                                if _stg >= 3:
                                    fw.op("dve", lambda e: e.tensor_tensor(out=st[:, 0:n], in0=t1_t[:, 0:n], in1=t2_t[:, 0:n], op=ALU.add),
                                          reads=[t1_t, t2_t], writes=[st])
                                else:
                                    fw.op("dve", lambda e: e.tensor_copy(out=st[:, 0:n], in_=t1_t[:, 0:n]), reads=[t1_t], writes=[st])
                                if kind == "ropek":
                                    fw.dma("sp", KsT, KsT.ap[idx2][:, tg:tg + n], st, st[:, 0:n])
                                else:
                                    for (ts, ln, os_) in own_segments(tg, n):
                                        fw.dma("sp", QsT, QsT.ap[idx2][:, os_:os_ + ln], st, st[:, ts - tg:ts - tg + ln])
                            deferred.append(part2)
                        elif kind in ("natq", "natk"):
                            st = s16[cnt["s16"] % 4]
                            cnt["s16"] += 1
                            fw.op("act", lambda e, st=st, A=A, n=n: e.copy(out=st[:, 0:n], in_=A[:, 0:n]), reads=[A], writes=[st])
                            if kind == "natk":
                                fw.dma("sp", KnT, KnT.ap[idx2][:, tg:tg + n], st, st[:, 0:n])
                            else:
                                for (ts, ln, os_) in own_segments(tg, n):
                                    fw.dma("sp", QnT, QnT.ap[idx2][:, os_:os_ + ln], st, st[:, ts - tg:ts - tg + ln])
                        else:
                            st = s32[cnt["s32"] % 3]
                            cnt["s32"] += 1
                            fw.op("act", lambda e, st=st, A=A, n=n: e.copy(out=st[:, 0:n], in_=A[:, 0:n]), reads=[A], writes=[st])
                            fw.dma("sp", cvT, cvT.ap[idx2][:, tg:tg + n], st, st[:, 0:n])
            run_deferred()
        fw.barrier()
        fw.pop()

    def attention_block(rhsQ, n_free, keys, Kt, Vt, pS, pO, pDn, Pb, cnt, masks, finalize):
        O = pO[cnt["o"] % 2]
        Dn = pDn[cnt["o"] % 2]
        cnt["o"] += 1
        pend = []
        nk = len(keys)
        for idx, (kt, mlist) in enumerate(keys):
            S_ = pS[cnt["s"] % len(pS)]
            P_ = Pb[cnt["s"] % len(Pb)]
            cnt["s"] += 1
            fw.mm(S_, [lambda e, S_=S_, kt=kt: e.matmul(S_[:, 0:n_free], lhsT=Kt[:, kt * 128:(kt + 1) * 128], rhs=rhsQ,
                                                        start=True, stop=True)], reads=[Kt] + masks["qtiles"])
            while pend:
                pend.pop(0)()
            fw.op("act", lambda e, S_=S_, P_=P_, kt=kt: e.activation(out=P_[:, 0:n_free], in_=S_[:, 0:n_free], func=AF.Exp,
                                                                     scale=SCL, bias=kvb_s[:, kt:kt + 1]),
                  reads=[S_, kvb_s], writes=[P_])
            for (mt, map_) in mlist:
                fw.op("dve", lambda e, P_=P_, map_=map_: e.tensor_tensor(out=P_[:, 0:n_free], in0=P_[:, 0:n_free], in1=map_, op=ALU.mult),
                      reads=[P_, mt], writes=[P_])

            def pv(P_=P_, kt=kt, idx=idx):
                fw.mm(O, [lambda e: e.matmul(O[:, 0:n_free], lhsT=Vt[:, kt, :], rhs=P_[:, 0:n_free], start=(idx == 0), stop=(idx == nk - 1))],
                      reads=[Vt, P_])
                fw.mm(Dn, [lambda e: e.matmul(Dn[:, 0:n_free], lhsT=ones_b[:], rhs=P_[:, 0:n_free], start=(idx == 0), stop=(idx == nk - 1))],
                      reads=[ones_b, P_])
            pend.append(pv)
        while pend:
            pend.pop(0)()
        finalize(O, Dn)

    if "p2a" in phases or "p2b" in phases:
        fw.new_phase("p2")
        fw.push()
        pS = [fw.psum(f"pS{i}", [128, 512]) for i in range(4)]
        pO = [fw.psum(f"pO{i}", [128, 512]) for i in range(2)]
        pDn = [fw.psum(f"pDn{i}", [128, 512]) for i in range(2)]
        Pb = [fw.sbuf(f"Pb{i}", [128, 512], BF16) for i in range(4)]
        dsb = [fw.sbuf(f"dsb{i}", [128, 512], F32) for i in range(2)]
        cnt = {"s": 0, "o": 0, "d": 0}

    if "p2a" in phases:
        fw.push()
        sm = fw.sbuf("sm", [128, 2, 512], BF16, dma=True)
        es = fw.sbuf("es", [128, 16], F32, dma=True)
        esx = fw.sbuf("esx", [128, 16, 128], F32)
        Kg = [fw.sbuf(f"Kg{i}", [128, TOK], BF16, dma=True) for i in range(2)]
        Vg = [fw.sbuf(f"Vg{i}", [128, 22, 128], BF16, dma=True) for i in range(2)]
        Qg = [fw.sbuf(f"Qg{i}", [128, 4, OWN], BF16, dma=True) for i in range(2)]
        yst = [fw.sbuf(f"yst{i}", [128, 4, OWN], BF16, dma=True) for i in range(2)]
        fw.dma("sp", sm, sm[:], swam, swam.ap)
        fw.dma("sp", es, es[:], sink, sink.ap.broadcast_to([128, 16]))
        fw.op("act", lambda e: e.activation(out=es[:], in_=es[:], func=AF.Exp), reads=[es], writes=[es])
        fw.op("dve", lambda e: e.tensor_copy(out=esx[:], in_=es[:, :].unsqueeze(2).broadcast_to([128, 16, 128])), reads=[es], writes=[esx])
        for g in range(4):
            K_t, V_t, Q_t, y_t = Kg[g % 2], Vg[g % 2], Qg[g % 2], yst[g % 2]
            fw.dma("sp", K_t, K_t[:], KsT, KsT.ap[g])
            fw.dma("sp", V_t, V_t[:], Vs, Vs.ap[:, :, g * 128:(g + 1) * 128].rearrange("t k d -> k t d"))
            fw.dma("sp", Q_t, Q_t[:], QsT, QsT.ap[4 * g:4 * g + 4].rearrange("h d o -> d h o"))
            for blk in range(18):
                if blk < 16:
                    qt = blk + 2
                    keys = [(qt - 1, [(sm, sm[:, 0, :])]), (qt, []), (qt + 1, [(sm, sm[:, 1, :])]), (20, []), (21, [])]
                    oc = blk * 128
                else:
                    keys = [(20, []), (21, [])]
                    oc = 2048 + (blk - 16) * 128
                rhsQ = Q_t[:, :, oc:oc + 128]

                def fin(O, Dn, g=g, oc=oc, y_t=y_t):
                    d_ = dsb[cnt["d"] % 2]
                    cnt["d"] += 1
                    fw.op("dve", lambda e: e.tensor_tensor(out=d_[:].rearrange("p (h q) -> p h q", h=4), in0=Dn[:].rearrange("p (h q) -> p h q", h=4),
                                                           in1=esx[:, 4 * g:4 * g + 4, :], op=ALU.add), reads=[Dn, esx], writes=[d_])
                    fw.op("dve", lambda e: e.reciprocal(out=d_[:], in_=d_[:]), reads=[d_], writes=[d_])
                    fw.op("dve", lambda e: e.tensor_tensor(out=y_t[:, :, oc:oc + 128], in0=O[:].rearrange("p (h q) -> p h q", h=4),
                                                           in1=d_[:].rearrange("p (h q) -> p h q", h=4), op=ALU.mult), reads=[O, d_], writes=[y_t])
                attention_block_swa(fw, rhsQ, keys, K_t, V_t, Q_t, pS, pO, pDn, Pb, cnt, kvb_s, ones_b, fin)
            fw.dma("sp", yT, yT.ap[4 * g:4 * g + 4].rearrange("c p o -> p c o"), y_t, y_t[:])
        fw.barrier()
        fw.pop()

    if "p2b" in phases:
        fw.push()
        nm = fw.sbuf("nm", [128, 4, 8, 512], BF16, dma=True)
        nbf = [fw.sbuf(f"nbf{i}", [128, 8, 512], F32, dma=True) for i in range(1)]
        Eh = [fw.sbuf(f"Eh{i}", [128, 8, 512], BF16) for i in range(2)]
        Kh = [fw.sbuf(f"Kh{i}", [128, TOK], BF16, dma=True) for i in range(2)]
        Vh = [fw.sbuf(f"Vh{i}", [128, 22, 128], BF16, dma=True) for i in range(2)]
        Qh = [fw.sbuf(f"Qh{i}", [128, OWN], BF16, dma=True) for i in range(2)]
        ysh = [fw.sbuf(f"ysh{i}", [128, OWN], BF16, dma=True) for i in range(2)]
        fw.dma("sp", nm, nm[:], natm, natm.ap)
        for h in range(8):
            K_t, V_t, Q_t, y_t, E_t = Kh[h % 2], Vh[h % 2], Qh[h % 2], ysh[h % 2], Eh[h % 2]
            nb_t = nbf[0]
            fw.dma("sp", K_t, K_t[:], KnT, KnT.ap[h])
            fw.dma("sp", V_t, V_t[:], Vn, Vn.ap[:, :, h * 128:(h + 1) * 128].rearrange("t k d -> k t d"))
            fw.dma("sp", Q_t, Q_t[:], QnT, QnT.ap[h])
            fw.dma("sp", nb_t, nb_t[:], natb, natb.ap[h])
            fw.op("act", lambda e, E_t=E_t, nb_t=nb_t: e.activation(out=E_t[:], in_=nb_t[:], func=AF.Exp), reads=[nb_t], writes=[E_t])
            for blk in range(5):
                if blk < 4:
                    keys = [(4 * blk + j, [(E_t, E_t[:, j, :]), (nm, nm[:, blk, j, :])]) for j in range(8)] + [(20, []), (21, [])]
                    oc, nf = blk * 512, 512
                else:
                    keys = [(20, []), (21, [])]
                    oc, nf = 2048, 256
                rhsQ = Q_t[:, oc:oc + nf]

                def fin(O, Dn, oc=oc, nf=nf, y_t=y_t):
                    d_ = dsb[cnt["d"] % 2]
                    cnt["d"] += 1
                    fw.op("dve", lambda e: e.reciprocal(out=d_[:, 0:nf], in_=Dn[:, 0:nf]), reads=[Dn], writes=[d_])
                    fw.op("dve", lambda e: e.tensor_tensor(out=y_t[:, oc:oc + nf], in0=O[:, 0:nf], in1=d_[:, 0:nf], op=ALU.mult),
                          reads=[O, d_], writes=[y_t])
                attention_block(rhsQ, nf, keys, K_t, V_t, pS, pO, pDn, Pb, cnt, {"qtiles": [Q_t]}, fin)
            fw.dma("sp", yT, yT.ap[24 + h], y_t, y_t[:])
        fw.barrier()
        fw.pop()

    if "p2a" in phases or "p2b" in phases:
        fw.barrier()
        fw.pop()

    if "p2c" in phases:
        fw.new_phase("p2c")
        fw.push()
        cp = fw.sbuf("cp", [128, 8, 4], F32, dma=True)
        ce = fw.sbuf("ce", [128, 2], F32, dma=True)
        ub = [fw.sbuf(f"ub{i}", [128, TOK], F32, dma=True) for i in range(2)]
        cb = [fw.sbuf(f"cb{i}", [128, TOK], F32, dma=True) for i in range(2)]
        bbuf = [fw.sbuf(f"bbuf{i}", [128, TOK], F32, dma=True) for i in range(2)]
        acc = [fw.sbuf(f"acc{i}", [128, OWN], F32) for i in range(1)]
        ycv = [fw.sbuf(f"ycv{i}", [128, OWN], BF16, dma=True) for i in range(2)]
        fw.dma("sp", cp, cp[:], convp, convp.ap)
        fw.dma("sp", ce, ce[:], cedge, cedge.ap)
        for cc in range(8):
            u_t, c_t, b_t, a_t, y_t = ub[cc % 2], cb[cc % 2], bbuf[cc % 2], acc[0], ycv[cc % 2]
            fw.dma("sp", u_t, u_t[:], cvT, cvT.ap[cc])
            fw.dma("sp", b_t, b_t[:], cvT, cvT.ap[8 + cc])
            fw.dma("sp", c_t, c_t[:], cvT, cvT.ap[16 + cc])
            fw.op("dve", lambda e: e.tensor_tensor(out=u_t[:], in0=u_t[:], in1=c_t[:], op=ALU.mult), reads=[u_t, c_t], writes=[u_t])
            fw.op("dve", lambda e: e.tensor_scalar(out=u_t[:, HALO - 1:HALO], in0=u_t[:, HALO - 1:HALO], scalar1=ce[:, 0:1], scalar2=None, op0=ALU.mult),
                  reads=[u_t, ce], writes=[u_t])
            fw.op("dve", lambda e: e.tensor_scalar(out=u_t[:, HALO + 2048:HALO + 2049], in0=u_t[:, HALO + 2048:HALO + 2049], scalar1=ce[:, 1:2],
                                                   scalar2=None, op0=ALU.mult), reads=[u_t, ce], writes=[u_t])
            w0, w1, w2, bb_ = cp[:, cc, 0:1], cp[:, cc, 1:2], cp[:, cc, 2:3], cp[:, cc, 3:4]
            L0, L1 = HALO, HALO + 2048
            fw.op("act", lambda e: e.activation(out=a_t[:, 0:2048], in_=u_t[:, L0:L1], func=AF.Identity, scale=w1, bias=bb_),
                  reads=[u_t, cp], writes=[a_t])
            fw.op("dve", lambda e: e.scalar_tensor_tensor(out=a_t[:, 0:2048], in0=u_t[:, L0 - 1:L1 - 1], scalar=w0, in1=a_t[:, 0:2048],
                                                          op0=ALU.mult, op1=ALU.add), reads=[u_t, a_t, cp], writes=[a_t])
            fw.op("dve", lambda e: e.scalar_tensor_tensor(out=a_t[:, 0:2048], in0=u_t[:, L0 + 1:L1 + 1], scalar=w2, in1=a_t[:, 0:2048],
                                                          op0=ALU.mult, op1=ALU.add), reads=[u_t, a_t, cp], writes=[a_t])
            C0, C1 = EXT, TOK
            fw.op("act", lambda e: e.activation(out=a_t[:, 2048:OWN], in_=u_t[:, C0:C1], func=AF.Identity, scale=w1, bias=bb_),
                  reads=[u_t, cp], writes=[a_t])
            fw.op("dve", lambda e: e.scalar_tensor_tensor(out=a_t[:, 2049:OWN], in0=u_t[:, C0:C1 - 1], scalar=w0, in1=a_t[:, 2049:OWN],
                                                          op0=ALU.mult, op1=ALU.add), reads=[u_t, a_t, cp], writes=[a_t])
            fw.op("dve", lambda e: e.scalar_tensor_tensor(out=a_t[:, 2048:OWN - 1], in0=u_t[:, C0 + 1:C1], scalar=w2, in1=a_t[:, 2048:OWN - 1],
                                                          op0=ALU.mult, op1=ALU.add), reads=[u_t, a_t, cp], writes=[a_t])
            fw.op("dve", lambda e: e.tensor_tensor(out=y_t[:, 0:2048], in0=a_t[:, 0:2048], in1=b_t[:, L0:L1], op=ALU.mult),
                  reads=[a_t, b_t], writes=[y_t])
            fw.op("dve", lambda e: e.tensor_tensor(out=y_t[:, 2048:OWN], in0=a_t[:, 2048:OWN], in1=b_t[:, C0:C1], op=ALU.mult),
                  reads=[a_t, b_t], writes=[y_t])
            fw.dma("sp", yT, yT.ap[16 + cc], y_t, y_t[:])
        fw.barrier()
        fw.pop()

    def ln_finalize(s1, s2, chunks, o_base, st_dram, tmp_a, tmp_b):
        for ci, (q0, n, isc) in enumerate(chunks):
            mean, rstd = tmp_a[ci % len(tmp_a)], tmp_b[ci % len(tmp_b)]
            fw.op("act", lambda e: e.mul(out=mean[:, 0:n], in_=s1[ci][:, 0:n], mul=1.0 / D), reads=[s1[ci]], writes=[mean])
            fw.op("dve", lambda e: e.tensor_tensor(out=rstd[:, 0:n], in0=mean[:, 0:n], in1=mean[:, 0:n], op=ALU.mult), reads=[mean], writes=[rstd])
            fw.op("dve", lambda e: e.scalar_tensor_tensor(out=rstd[:, 0:n], in0=s2[ci][:, 0:n], scalar=1.0 / D, in1=rstd[:, 0:n],
                                                          op0=ALU.mult, op1=ALU.subtract), reads=[s2[ci], rstd], writes=[rstd])
            fw.op("dve", lambda e: e.tensor_scalar(out=rstd[:, 0:n], in0=rstd[:, 0:n], scalar1=LN_EPS_P, scalar2=None, op0=ALU.add),
                  reads=[rstd], writes=[rstd])
            fw.op("dve", lambda e: e.reciprocal(out=rstd[:, 0:n], in_=rstd[:, 0:n]), reads=[rstd], writes=[rstd])
            fw.op("act", lambda e: e.activation(out=rstd[:, 0:n], in_=rstd[:, 0:n], func=AF.Sqrt), reads=[rstd], writes=[rstd])
            fw.dma("sp", st_dram, st_dram.ap[0][:, o_base + q0:o_base + q0 + n], mean, mean[:, 0:n])
            fw.dma("sp", st_dram, st_dram.ap[1][:, o_base + q0:o_base + q0 + n], rstd, rstd[:, 0:n])

    if "p3" in phases:
        fw.new_phase("p3")
        fw.push()
        SB3 = 1152
        ySB = fw.sbuf("ySB", [128, 32, SB3], BF16, dma=True)
        rinv = fw.sbuf("rinv", [128, 3, SB3], F32)
        sq = [fw.sbuf(f"sq{i}", [128, 512], BF16) for i in range(3)]
        wt = [fw.sbuf(f"wo{i}", [128, 32, 512], BF16, dma=True) for i in range(2)]
        hres = [fw.sbuf(f"hres{i}", [128, SB3], F32, dma=True) for i in range(2)]
        zt = [fw.sbuf(f"zt{i}", [128, 512], F32, dma=True) for i in range(3)]
        zq = [fw.sbuf(f"zq{i}", [128, 512], F32) for i in range(3)]
        mtmp = [fw.sbuf(f"mtmp{i}", [128, 512], F32, dma=True) for i in range(2)]
        rtmp = [fw.sbuf(f"rtmp{i}", [128, 512], F32, dma=True) for i in range(2)]
        pA = [fw.psum(f"p3A{i}", [128, 512]) for i in range(2)]
        ps1 = [fw.psum(f"p3s1{i}", [128, 512]) for i in range(3)]
        ps2 = [fw.psum(f"p3s2{i}", [128, 512]) for i in range(3)]
        cnt = {"a": 0, "w": 0, "h": 0, "z": 0, "q": 0}
        groups = [(0, 16, 2048.0), (16, 24, 1024.0), (24, 32, 1024.0)]
        for sb in range(2):
            O0 = sb * SB3
            chunks = [(0, 512, False), (512, 512, False), (1024, 128, False)] if sb == 0 else \
                     [(0, 512, False), (512, 384, False), (896, 256, True)]
            for q4 in range(4):
                fw.dma("sp", ySB, ySB[:, q4 * 8:(q4 + 1) * 8, :], yT, yT.ap[q4 * 8:(q4 + 1) * 8, :, O0:O0 + SB3].rearrange("c p o -> p c o"))
            for gi, (c0, c1, width) in enumerate(groups):
                for (q0, n, isc) in chunks:
                    A = pA[cnt["a"] % 2]
                    cnt["a"] += 1
                    for c in range(c0, c1):
                        s_t = sq[cnt["q"] % 3]
                        cnt["q"] += 1
                        fw.op("act", lambda e, s_t=s_t, c=c, q0=q0, n=n: e.activation(out=s_t[:, 0:n], in_=ySB[:, c, q0:q0 + n], func=AF.Square),
                              reads=[ySB], writes=[s_t])
                        fw.mm(A, [lambda e, s_t=s_t, c=c, A=A, n=n, c0=c0, c1=c1: e.matmul(A[:, 0:n], lhsT=ones_b[:], rhs=s_t[:, 0:n],
                                                                                         start=(c == c0), stop=(c == c1 - 1))],
                              reads=[s_t, ones_b])
                    fw.op("dve", lambda e, A=A, gi=gi, q0=q0, n=n, width=width: e.tensor_scalar(
                        out=rinv[:, gi, q0:q0 + n], in0=A[:, 0:n], scalar1=1.0 / width, scalar2=RMS_EPS, op0=ALU.mult, op1=ALU.add),
                        reads=[A], writes=[rinv])
                    fw.op("dve", lambda e, gi=gi, q0=q0, n=n: e.reciprocal(out=rinv[:, gi, q0:q0 + n], in_=rinv[:, gi, q0:q0 + n]),
                          reads=[rinv], writes=[rinv])
                    fw.op("act", lambda e, gi=gi, q0=q0, n=n: e.activation(out=rinv[:, gi, q0:q0 + n], in_=rinv[:, gi, q0:q0 + n], func=AF.Sqrt),
                          reads=[rinv], writes=[rinv])
            for gi, (c0, c1, width) in enumerate(groups):
                for c in range(c0, c1):
                    fw.op("dve", lambda e, c=c, gi=gi: e.scalar_tensor_tensor(
                        out=ySB[:, c, :], in0=ySB[:, c, :], scalar=vc[:, 0, c:c + 1], in1=rinv[:, gi, :], op0=ALU.mult, op1=ALU.mult),
                        reads=[ySB, rinv, vc], writes=[ySB])
            pend = []
            for ct in range(8):
                w_t = wt[cnt["w"] % 2]
                cnt["w"] += 1
                fw.dma("pool", w_t, w_t[:], w_out, w_out.ap[:, ct * 512:(ct + 1) * 512].rearrange("(kc p) n -> p kc n", p=128))
                for sub in range(4):
                    dc = ct * 4 + sub
                    h_t = hres[cnt["h"] % 2]
                    cnt["h"] += 1
                    for (os_, ln, ts) in own_to_tok(O0, SB3):
                        fw.dma("sp", h_t, h_t[:, os_ - O0:os_ - O0 + ln], hT, hT.ap[dc][:, ts:ts + ln])
                    for ci, (q0, n, isc) in enumerate(chunks):
                        A = pA[cnt["a"] % 2]
                        cnt["a"] += 1
                        fw.mm(A, [(lambda e, kc=kc, A=A, sub=sub, q0=q0, n=n, w_t=w_t: e.matmul(
                            A[:, 0:n], lhsT=w_t[:, kc, sub * 128:(sub + 1) * 128], rhs=ySB[:, kc, q0:q0 + n],
                            start=(kc == 0), stop=(kc == 31))) for kc in range(32)], reads=[ySB, w_t])
                        while pend:
                            pend.pop(0)()
                        z_t = zt[cnt["z"] % 3]
                        q_t = zq[cnt["z"] % 3]
                        cnt["z"] += 1
                        fw.op("dve", lambda e, z_t=z_t, A=A, dc=dc, isc=isc, h_t=h_t, q0=q0, n=n: e.scalar_tensor_tensor(
                            out=z_t[:, 0:n], in0=A[:, 0:n], scalar=MO(2, isc, dc), in1=h_t[:, q0:q0 + n], op0=ALU.mult, op1=ALU.add),
                            reads=[A, h_t, mo], writes=[z_t])
                        fw.op("act", lambda e, z_t=z_t, q_t=q_t, n=n: e.activation(out=q_t[:, 0:n], in_=z_t[:, 0:n], func=AF.Square),
                              reads=[z_t], writes=[q_t])
                        fw.dma("sp", z1T, z1T.ap[dc][:, O0 + q0:O0 + q0 + n], z_t, z_t[:, 0:n])

                        def stats(z_t=z_t, q_t=q_t, ci=ci, n=n, dc=dc):
                            fw.mm(ps1[ci], [lambda e: e.matmul(ps1[ci][:, 0:n], lhsT=ones_f[:], rhs=z_t[:, 0:n], start=(dc == 0), stop=(dc == 31))],
                                  reads=[z_t, ones_f])
                            fw.mm(ps2[ci], [lambda e: e.matmul(ps2[ci][:, 0:n], lhsT=ones_f[:], rhs=q_t[:, 0:n], start=(dc == 0), stop=(dc == 31))],
                                  reads=[q_t, ones_f])
                        pend.append(stats)
            while pend:
                pend.pop(0)()
            ln_finalize(ps1, ps2, chunks, O0, st1, mtmp, rtmp)
        fw.barrier()
        fw.pop()

    def ln_apply(z_t, mean_t, rstd_t, gi, bi, out_t):
        fw.op("dve", lambda e: e.tensor_tensor(out=z_t[:], in0=z_t[:], in1=mean_t[:, :].unsqueeze(1).broadcast_to([128, 32, 128]), op=ALU.subtract),
              reads=[z_t, mean_t], writes=[z_t])
        fw.op("dve", lambda e: e.tensor_tensor(out=z_t[:], in0=z_t[:], in1=rstd_t[:, :].unsqueeze(1).broadcast_to([128, 32, 128]), op=ALU.mult),
              reads=[z_t, rstd_t], writes=[z_t])
        for kc in range(32):
            fw.op("act", lambda e, kc=kc: e.activation(out=out_t[:, kc, :], in_=z_t[:, kc, :], func=AF.Identity,
                                                       scale=vc[:, gi, kc:kc + 1], bias=vc[:, bi, kc:kc + 1]), reads=[z_t, vc], writes=[out_t])

    if "p4" in phases:
        fw.new_phase("p4")
        fw.push()
        SB4 = 768
        xm2 = fw.sbuf("xm2", [128, 32, SB4], BF16)
        hid = fw.sbuf("hid", [128, 32, SB4], BF16)
        gT = fw.sbuf("gT", [16, SB4], F32)
        wr_s = fw.sbuf("wr_s", [128, 32, 20], F32, dma=True)
        br_s = fw.sbuf("br_s", [128, 20], F32, dma=True)
        sel_s = fw.sbuf("sel_s", [16, 16, 128], F32, dma=True)
        id_s = fw.sbuf("id_s", [128, 128], F32, dma=True)
        fw.dma("sp", wr_s, wr_s[:], w_r, w_r.ap.rearrange("(kc p) n -> p kc n", p=128))
        fw.dma("sp", br_s, br_s[:], b_r, b_r.ap.broadcast_to([128, 20]))
        fw.dma("sp", sel_s, sel_s[:], sel, sel.ap)
        fw.dma("sp", id_s, id_s[:], ident, ident.ap)
        for sb in range(3):
            O0 = sb * SB4
            chunks = [(0, 512, False), (512, 256, sb == 2)]
            fw.push()
            zb2 = [fw.sbuf(f"zb{i}", [128, 32, 128], F32, dma=True) for i in range(2)]
            h1b2 = [fw.sbuf(f"h1b{i}", [128, 32, 128], F32, dma=True) for i in range(2)]
            xf = fw.sbuf("xf", [128, 32, 128], F32)
            mt2 = [fw.sbuf(f"mt_{i}", [128, 128], F32, dma=True) for i in range(2)]
            rt2 = [fw.sbuf(f"rt_{i}", [128, 128], F32, dma=True) for i in range(2)]
            rs = fw.sbuf("rs", [128, 64], F32)
            gates = fw.sbuf("gates", [128, 16], F32)
            pR = fw.psum("pR", [128, 512])
            pT = fw.psum("pT", [128, 512])
            for tt in range(6):
                o = O0 + tt * 128
                isc = (o >= 2048)
                zb, h1b, mt_, rt_ = zb2[tt % 2], h1b2[tt % 2], mt2[tt % 2], rt2[tt % 2]
                fw.dma("sp", zb, zb[:], z1T, z1T.ap[:, :, o:o + 128].rearrange("c p o -> p c o"))
                fw.dma("sp", mt_, mt_[:], st1, st1.ap[0][:, o:o + 128])
                fw.dma("sp", rt_, rt_[:], st1, st1.ap[1][:, o:o + 128])
                ln_apply(zb, mt_, rt_, 1, 2, h1b)
                fw.dma("sp", h1T, h1T.ap[:, :, o:o + 128].rearrange("c p o -> p c o"), h1b, h1b[:])
                mb = 6 if isc else 0
                fw.op("dve", lambda e, mb=mb: e.tensor_tensor(out=xf[:], in0=h1b[:], in1=mo[:, mb + 4, :].unsqueeze(2).broadcast_to([128, 32, 128]), op=ALU.mult),
                      reads=[h1b, mo], writes=[xf])
                fw.op("dve", lambda e, mb=mb: e.tensor_tensor(out=xf[:], in0=xf[:], in1=mo[:, mb + 3, :].unsqueeze(2).broadcast_to([128, 32, 128]), op=ALU.add),
                      reads=[xf, mo], writes=[xf])
                fw.op("act", lambda e, tt=tt: e.copy(out=xm2[:, :, tt * 128:(tt + 1) * 128], in_=xf[:]), reads=[xf], writes=[xm2])
                fw.mm(pR, [(lambda e, kc=kc: e.matmul(pR[:, 0:20], lhsT=xf[:, kc, :], rhs=wr_s[:, kc, :], start=(kc == 0), stop=(kc == 31)))
                           for kc in range(32)], reads=[xf, wr_s])
                LG, MX, NMX, EX, SM, GP, OH, LE, E3, EIN, M1, OH1, E2, M2, OH2, DF, W1, W2, WI = (
                    rs[:, 0:20], rs[:, 20:21], rs[:, 21:22], rs[:, 22:26], rs[:, 26:27], rs[:, 27:28], rs[:, 28:32], rs[:, 4:20],
                    rs[:, 32:48], rs[:, 48:52], rs[:, 52:53], rs[:, 53:57], rs[:, 57:61], rs[:, 61:62], rs[:, 22:26], rs[:, 62:63],
                    rs[:, 63:64], rs[:, 26:27], rs[:, 48:52])

                def V(fn, reads=(rs,), writes=(rs,)):
                    fw.op("dve", fn, reads=list(reads), writes=list(writes))
                V(lambda e: e.tensor_tensor(out=LG, in0=pR[:, 0:20], in1=br_s[:], op=ALU.add), reads=(pR, br_s))
                V(lambda e: e.reduce_max(out=MX, in_=rs[:, 0:4], axis=mybir.AxisListType.X))
                V(lambda e: e.tensor_scalar(out=OH, in0=rs[:, 0:4], scalar1=MX, scalar2=None, op0=ALU.is_equal))
                V(lambda e: e.tensor_scalar(out=NMX, in0=MX, scalar1=-1.0, scalar2=None, op0=ALU.mult))
                fw.op("act", lambda e: e.activation(out=EX, in_=rs[:, 0:4], func=AF.Exp, bias=NMX, scale=1.0), reads=[rs], writes=[rs])
                V(lambda e: e.reduce_sum(out=SM, in_=EX, axis=mybir.AxisListType.X))
                V(lambda e: e.reciprocal(out=GP, in_=SM))
                V(lambda e: e.tensor_tensor(out=E3.rearrange("p (g e) -> p g e", g=4), in0=LE.rearrange("p (g e) -> p g e", g=4),
                                            in1=OH.unsqueeze(2).broadcast_to([128, 4, 4]), op=ALU.mult))
                V(lambda e: e.tensor_tensor(out=EIN, in0=rs[:, 32:36], in1=rs[:, 36:40], op=ALU.add))
                V(lambda e: e.tensor_tensor(out=EIN, in0=EIN, in1=rs[:, 40:44], op=ALU.add))
                V(lambda e: e.tensor_tensor(out=EIN, in0=EIN, in1=rs[:, 44:48], op=ALU.add))
                V(lambda e: e.reduce_max(out=M1, in_=EIN, axis=mybir.AxisListType.X))
                V(lambda e: e.tensor_scalar(out=OH1, in0=EIN, scalar1=M1, scalar2=None, op0=ALU.is_equal))
                V(lambda e: e.scalar_tensor_tensor(out=E2, in0=OH1, scalar=-1e30, in1=EIN, op0=ALU.mult, op1=ALU.add))
                V(lambda e: e.reduce_max(out=M2, in_=E2, axis=mybir.AxisListType.X))
                V(lambda e: e.tensor_scalar(out=OH2, in0=E2, scalar1=M2, scalar2=None, op0=ALU.is_equal))
                V(lambda e: e.tensor_tensor(out=DF, in0=M2, in1=M1, op=ALU.subtract))
                fw.op("act", lambda e: e.activation(out=DF, in_=DF, func=AF.Exp), reads=[rs], writes=[rs])
                V(lambda e: e.tensor_scalar(out=W1, in0=DF, scalar1=1.0, scalar2=None, op0=ALU.add))
                V(lambda e: e.reciprocal(out=W1, in_=W1))
                V(lambda e: e.tensor_tensor(out=W2, in0=DF, in1=W1, op=ALU.mult))
                V(lambda e: e.tensor_tensor(out=W1, in0=W1, in1=GP, op=ALU.mult))
                V(lambda e: e.tensor_tensor(out=W2, in0=W2, in1=GP, op=ALU.mult))
                V(lambda e: e.tensor_scalar(out=WI, in0=OH1, scalar1=W1, scalar2=None, op0=ALU.mult))
                V(lambda e: e.scalar_tensor_tensor(out=WI, in0=OH2, scalar=W2, in1=WI, op0=ALU.mult, op1=ALU.add))
                fw.op("dve", lambda e: e.tensor_tensor(out=gates[:].rearrange("p (g e) -> p g e", g=4),
                                                       in0=OH.unsqueeze(2).broadcast_to([128, 4, 4]),
                                                       in1=WI.unsqueeze(1).broadcast_to([128, 4, 4]), op=ALU.mult), reads=[rs], writes=[gates])
                fw.mm(pT, [lambda e: e.transpose(out=pT[0:16, 0:128], in_=gates[:], identity=id_s[:])], reads=[gates, id_s])
                fw.op("act", lambda e, tt=tt: e.copy(out=gT[:, tt * 128:(tt + 1) * 128], in_=pT[0:16, 0:128]), reads=[pT], writes=[gT])
            fw.barrier()
            fw.pop()
            fw.push()
            wgu = [fw.sbuf(f"wgu{i}", [128, 32, 256], BF16, dma=True) for i in range(3)]
            gbc = [fw.sbuf(f"gbc{i}", [128, SB4], F32) for i in range(2)]
            sgt = [fw.sbuf(f"sgt{i}", [128, 512], F32) for i in range(2)]
            hres = [fw.sbuf(f"hres4{i}", [128, SB4], F32, dma=True) for i in range(2)]
            zt = [fw.sbuf(f"zt4{i}", [128, 512], F32, dma=True) for i in range(3)]
            zq = [fw.sbuf(f"zq4{i}", [128, 512], F32) for i in range(3)]
            mtmp = [fw.sbuf(f"mtmp4{i}", [128, 512], F32, dma=True) for i in range(2)]
            rtmp = [fw.sbuf(f"rtmp4{i}", [128, 512], F32, dma=True) for i in range(2)]
            pG = [fw.psum(f"pG{i}", [128, 512]) for i in range(2)]
            pU = [fw.psum(f"pU{i}", [128, 512]) for i in range(2)]
            ps1 = [fw.psum(f"p4s1{i}", [128, 512]) for i in range(2)]
            ps2 = [fw.psum(f"p4s2{i}", [128, 512]) for i in range(2)]
            cw = 0
            cg = 0
            for ex in range(16):
                wg_t = wgu[cw % 3]
                wu_t = wgu[(cw + 1) % 3]
                cw += 2
                fw.dma("pool", wg_t, wg_t[:], w_gate, w_gate.ap[ex].rearrange("(kc p) n -> p kc n", p=128))
                fw.dma("pool", wu_t, wu_t[:], w_up, w_up.ap[ex].rearrange("(kc p) n -> p kc n", p=128))
                g_t = gbc[ex % 2]
                for (q0, n, isc) in chunks:
                    Gp = pG[cg % 2]
                    fw.mm(Gp, [lambda e, Gp=Gp, q0=q0, n=n, ex=ex: e.matmul(Gp[:, 0:n], lhsT=sel_s[:, ex, :], rhs=gT[:, q0:q0 + n], start=True, stop=True)],
                          reads=[sel_s, gT])
                    fw.op("act", lambda e, Gp=Gp, g_t=g_t, q0=q0, n=n: e.copy(out=g_t[:, q0:q0 + n], in_=Gp[:, 0:n]), reads=[Gp], writes=[g_t])
                    cg += 1
                for fc in range(2):
                    for (q0, n, isc) in chunks:
                        Gp, Up = pG[cg % 2], pU[cg % 2]
                        s_t = sgt[cg % 2]
                        cg += 1
                        fw.mm(Gp, [(lambda e, kc=kc, Gp=Gp, fc=fc, q0=q0, n=n, wg_t=wg_t: e.matmul(
                            Gp[:, 0:n], lhsT=wg_t[:, kc, fc * 128:(fc + 1) * 128], rhs=xm2[:, kc, q0:q0 + n],
                            start=(kc == 0), stop=(kc == 31))) for kc in range(32)], reads=[xm2, wg_t])
                        fw.mm(Up, [(lambda e, kc=kc, Up=Up, fc=fc, q0=q0, n=n, wu_t=wu_t: e.matmul(
                            Up[:, 0:n], lhsT=wu_t[:, kc, fc * 128:(fc + 1) * 128], rhs=xm2[:, kc, q0:q0 + n],
                            start=(kc == 0), stop=(kc == 31))) for kc in range(32)], reads=[xm2, wu_t])
                        fw.op("act", lambda e, Gp=Gp, s_t=s_t, n=n: e.activation(out=s_t[:, 0:n], in_=Gp[:, 0:n], func=AF.Silu), reads=[Gp], writes=[s_t])
                        fw.op("dve", lambda e, Up=Up, s_t=s_t, n=n: e.tensor_tensor(out=s_t[:, 0:n], in0=Up[:, 0:n], in1=s_t[:, 0:n], op=ALU.mult),
                              reads=[Up, s_t], writes=[s_t])
                        fw.op("dve", lambda e, s_t=s_t, g_t=g_t, ex=ex, fc=fc, q0=q0, n=n: e.tensor_tensor(
                            out=hid[:, ex * 2 + fc, q0:q0 + n], in0=s_t[:, 0:n], in1=g_t[:, q0:q0 + n], op=ALU.mult),
                            reads=[s_t, g_t], writes=[hid])
            pend = []
            ca = 0
            cz = 0
            for ct in range(16):
                w_t = wgu[cw % 3]
                cw += 1
                fw.dma("pool", w_t, w_t[:], w_down, w_down.ap[:, ct * 256:(ct + 1) * 256].rearrange("(kc p) n -> p kc n", p=128))
                for sub in range(2):
                    dc = ct * 2 + sub
                    h_t = hres[dc % 2]
                    fw.dma("sp", h_t, h_t[:], h1T, h1T.ap[dc][:, O0:O0 + SB4])
                    for ci, (q0, n, isc) in enumerate(chunks):
                        A = pG[ca % 2] if (ca // 2) % 2 == 0 else pU[ca % 2]
                        ca += 1
                        fw.mm(A, [(lambda e, kc=kc, A=A, sub=sub, q0=q0, n=n, w_t=w_t: e.matmul(
                            A[:, 0:n], lhsT=w_t[:, kc, sub * 128:(sub + 1) * 128], rhs=hid[:, kc, q0:q0 + n],
                            start=(kc == 0), stop=(kc == 31))) for kc in range(32)], reads=[hid, w_t])
                        while pend:
                            pend.pop(0)()
                        z_t, q_t = zt[cz % 3], zq[cz % 3]
                        cz += 1
                        fw.op("dve", lambda e, z_t=z_t, A=A, dc=dc, isc=isc, h_t=h_t, q0=q0, n=n: e.scalar_tensor_tensor(
                            out=z_t[:, 0:n], in0=A[:, 0:n], scalar=MO(5, isc, dc), in1=h_t[:, q0:q0 + n], op0=ALU.mult, op1=ALU.add),
                            reads=[A, h_t, mo], writes=[z_t])
                        fw.op("act", lambda e, z_t=z_t, q_t=q_t, n=n: e.activation(out=q_t[:, 0:n], in_=z_t[:, 0:n], func=AF.Square),
                              reads=[z_t], writes=[q_t])
                        fw.dma("sp", z2T, z2T.ap[dc][:, O0 + q0:O0 + q0 + n], z_t, z_t[:, 0:n])

                        def stats(z_t=z_t, q_t=q_t, ci=ci, n=n, dc=dc):
                            fw.mm(ps1[ci], [lambda e: e.matmul(ps1[ci][:, 0:n], lhsT=ones_f[:], rhs=z_t[:, 0:n], start=(dc == 0), stop=(dc == 31))],
                                  reads=[z_t, ones_f])
                            fw.mm(ps2[ci], [lambda e: e.matmul(ps2[ci][:, 0:n], lhsT=ones_f[:], rhs=q_t[:, 0:n], start=(dc == 0), stop=(dc == 31))],
                                  reads=[q_t, ones_f])
                        pend.append(stats)
            while pend:
                pend.pop(0)()
            ln_finalize(ps1, ps2, chunks, O0, st2, mtmp, rtmp)
            fw.barrier()
            fw.pop()
        fw.barrier()
        fw.pop()

    if "p5" in phases:
        fw.new_phase("p5")
        fw.push()
        zb = [fw.sbuf(f"zb5{i}", [128, 8, 512], F32, dma=True) for i in range(2)]
        ob = [fw.sbuf(f"ob5{i}", [128, 8, 512], F32, dma=True) for i in range(2)]
        mt_ = [fw.sbuf(f"mt5{i}", [128, 512], F32, dma=True) for i in range(2)]
        rt_ = [fw.sbuf(f"rt5{i}", [128, 512], F32, dma=True) for i in range(2)]
        i5 = 0
        for ci, (o, n) in enumerate([(0, 512), (512, 512), (1024, 512), (1536, 512), (2048, 256)]):
            m_t, r_t = mt_[ci % 2], rt_[ci % 2]
            fw.dma("sp", m_t, m_t[:, 0:n], st2, st2.ap[0][:, o:o + n])
            fw.dma("sp", r_t, r_t[:, 0:n], st2, st2.ap[1][:, o:o + n])
            for kq in range(4):
                z_t, o_t = zb[i5 % 2], ob[i5 % 2]
                i5 += 1
                fw.dma("sp", z_t, z_t[:, :, 0:n], z2T, z2T.ap[kq * 8:(kq + 1) * 8, :, o:o + n].rearrange("c p o -> p c o"))
                fw.op("dve", lambda e, z_t=z_t, m_t=m_t, n=n: e.tensor_tensor(
                    out=z_t[:, :, 0:n], in0=z_t[:, :, 0:n], in1=m_t[:, 0:n].unsqueeze(1).broadcast_to([128, 8, n]), op=ALU.subtract),
                    reads=[z_t, m_t], writes=[z_t])
                fw.op("dve", lambda e, z_t=z_t, r_t=r_t, n=n: e.tensor_tensor(
                    out=z_t[:, :, 0:n], in0=z_t[:, :, 0:n], in1=r_t[:, 0:n].unsqueeze(1).broadcast_to([128, 8, n]), op=ALU.mult),
                    reads=[z_t, r_t], writes=[z_t])
                for k8 in range(8):
                    kc = kq * 8 + k8
                    fw.op("act", lambda e, z_t=z_t, o_t=o_t, k8=k8, kc=kc, n=n: e.activation(
                        out=o_t[:, k8, 0:n], in_=z_t[:, k8, 0:n], func=AF.Identity, scale=vc[:, 3, kc:kc + 1], bias=vc[:, 4, kc:kc + 1]),
                        reads=[z_t, vc], writes=[o_t])
                fw.dma("sp", hout, hout.ap[kq * 8:(kq + 1) * 8, :, o:o + n].rearrange("c p o -> p c o"), o_t, o_t[:, :, 0:n])
        fw.barrier()
        fw.pop()

    fw.barrier()
    fw.wait("sp", hout.w)
    fw.close()
    return nc, fw


def attention_block_swa(fw, rhsQ, keys, K_t, V_t, Q_t, pS, pO, pDn, Pb, cnt, kvb_s, ones_b, finalize):
    O = pO[cnt["o"] % 2]
    Dn = pDn[cnt["o"] % 2]
    cnt["o"] += 1
    pend = []
    nk = len(keys)
    r4 = lambda ap: ap.rearrange("p (h q) -> p h q", h=4)
    for idx, (kt, mlist) in enumerate(keys):
        S_ = pS[cnt["s"] % len(pS)]
        P_ = Pb[cnt["s"] % len(Pb)]
        cnt["s"] += 1
        fw.mm(S_, [lambda e, S_=S_, kt=kt: e.matmul(r4(S_[:]), lhsT=K_t[:, kt * 128:(kt + 1) * 128], rhs=rhsQ, start=True, stop=True)],
              reads=[K_t, Q_t])
        while pend:
            pend.pop(0)()
        fw.op("act", lambda e, S_=S_, P_=P_, kt=kt: e.activation(out=P_[:], in_=S_[:], func=AF.Exp, scale=SCL, bias=kvb_s[:, kt:kt + 1]),
              reads=[S_, kvb_s], writes=[P_])
        for (mt, map_) in mlist:
            fw.op("dve", lambda e, P_=P_, map_=map_: e.tensor_tensor(out=P_[:], in0=P_[:], in1=map_, op=ALU.mult), reads=[P_, mt], writes=[P_])

        def pv(P_=P_, kt=kt, idx=idx):
            fw.mm(O, [lambda e: e.matmul(O[:], lhsT=V_t[:, kt, :], rhs=P_[:], start=(idx == 0), stop=(idx == nk - 1))], reads=[V_t, P_])
            fw.mm(Dn, [lambda e: e.matmul(Dn[:], lhsT=ones_b[:], rhs=P_[:], start=(idx == 0), stop=(idx == nk - 1))], reads=[ones_b, P_])
        pend.append(pv)
    while pend:
        pend.pop(0)()
    finalize(O, Dn)


def fm(vec):
    v = np.asarray(vec, np.float32)
    lead = v.shape[:-1]
    v = v.reshape(lead + (32, 128))
    return np.ascontiguousarray(np.moveaxis(v, -1, 0))


def const_tables(half):
    s0 = half * 2048
    t = np.arange(EXT)
    s = s0 - HALO + t
    valid = (s >= 0) & (s < S)
    sc = np.clip(s, 0, S - 1)
    pos = np.stack([sc // 64, sc % 64], -1).astype(np.float32)
    inv_freq = (np.float32(10000.0) ** (-np.arange(32, dtype=np.float32) / np.float32(32))).astype(np.float32)
    ang = (pos[:, :, None] * inv_freq).astype(np.float32)
    cs, sn = np.cos(ang).astype(np.float32), np.sin(ang).astype(np.float32)
    d = np.arange(128)
    axis, hf, f = d // 64, (d % 64) // 32, d % 32
    rope = np.zeros((2, 128, TOK), np.float32)
    rope[0, :, :EXT] = cs[:, axis, f].T
    rope[1, :, :EXT] = (sn[:, axis, f] * np.where(hf == 0, -1.0, 1.0)[None, :]).T
    rope[0, :, EXT:] = 1.0
    partner = np.where(hf == 0, d + 32, d - 32)
    perm = np.zeros((128, 128), np.float32)
    perm[partner, d] = 1.0
    kvb = np.zeros((128, 22), np.float32)
    kvb[:, :20] = np.where(valid, 0.0, NEGB).reshape(20, 128).T
    cedge = np.zeros((128, 2), np.float32)
    cedge[:, 0] = 1.0 if s0 - 1 >= 0 else 0.0
    cedge[:, 1] = 1.0 if s0 + 2048 < S else 0.0
    k = np.arange(128)[:, None]
    q = np.arange(128)[None, :]
    swam = np.zeros((128, 2, 4, 128), np.float32)
    swam[:, 0] = (k >= q)[:, None, :]
    swam[:, 1] = (k <= q)[:, None, :]
    swam = swam.reshape(128, 2, 512)
    R0 = half * 32
    natm = np.zeros((128, 4, 8, 512), np.float32)
    kk = np.arange(128)
    qq = np.arange(512)
    for i in range(4):
        for j in range(8):
            kr = R0 + 8 * i - 4 + 2 * j + kk // 64
            kc = kk % 64
            qr = R0 + 8 * i + qq // 64
            qc = qq % 64
            rs = np.clip(qr - 4, 0, 56)
            rowok = (kr[:, None] >= rs[None, :]) & (kr[:, None] <= rs[None, :] + 7) & (kr[:, None] >= 0) & (kr[:, None] < 64)
            cst = np.clip(qc - 8, 0, 48)
            colok = (kc[:, None] >= cst[None, :]) & (kc[:, None] < cst[None, :] + 16)
            natm[:, i, j, :] = rowok & colok
    sel = np.zeros((16, 16, 128), np.float32)
    for e in range(16):
        sel[e, e, :] = 1.0
    return dict(rope=rope, perm=perm.astype(ml_dtypes.bfloat16), kvb=kvb, cedge=cedge, swam=swam.astype(ml_dtypes.bfloat16),
                natm=natm.astype(ml_dtypes.bfloat16), sel=sel, ident=np.eye(128, dtype=np.float32))


def nat_bias_table(rpb):
    kk = np.arange(128)
    qq = np.arange(512)
    out = np.zeros((8, 128, 8, 512), np.float32)
    for j in range(8):
        dr = (-4 + 2 * j + kk // 64)[:, None] - (qq // 64)[None, :] + 7
        dc = (kk % 64)[:, None] - (qq % 64)[None, :] + 15
        out[:, :, j, :] = rpb[:, np.clip(dr, 0, 14), np.clip(dc, 0, 30)]
    return out


_CACHE = {}


def _get(name, fn):
    if name not in _CACHE:
        _CACHE[name] = fn()
    return _CACHE[name]


def run_ada(c, c_ctx, w_ada, b_ada):
    nc = _get("ada", build_ada)
    cc = np.zeros((8, D), np.float32)
    cc[:4] = c
    cc[4] = c_ctx
    cT = np.ascontiguousarray(cc.reshape(8, 32, 128).transpose(2, 1, 0))
    in_maps = [{"cT": cT, "wa": np.ascontiguousarray(w_ada[:, :, k * ADA_CS:(k + 1) * ADA_CS]),
                "ba": np.ascontiguousarray(b_ada[:, k * ADA_CS:(k + 1) * ADA_CS])} for k in range(8)]
    res = run_bass_kernel_spmd(nc, in_maps, core_ids=list(range(8)))
    return np.concatenate([r["mod"] for r in res.results], axis=2)


def layer_inputs(l, core, h, hc, mod, P, tabs):
    b, half = core // 2, core % 2
    s0 = half * 2048
    ext = np.zeros((TOK, D), np.float32)
    lo, hi = s0 - HALO, s0 + 2048 + HALO
    a, e = max(lo, 0), min(hi, S)
    ext[a - lo:e - lo] = h[b, a:e]
    ext[EXT:] = hc[b]
    hT = np.ascontiguousarray(ext.T.reshape(32, 128, TOK))
    mv = np.concatenate([fm(mod[l, b].reshape(6, D)), fm(mod[l, 4].reshape(6, D))], axis=1)
    t = tabs[half]
    d = dict(hT=hT, modv=np.ascontiguousarray(mv))
    d.update(P)
    d.update(t)
    return d


def layer_params(l, inp):
    P = {}
    P["w_in"] = np.ascontiguousarray(inp["w_in"][l])
    P["w_out"] = np.ascontiguousarray(inp["w_out"][l])
    P["w_gate"] = np.ascontiguousarray(inp["w_gate"][l])
    P["w_up"] = np.ascontiguousarray(inp["w_up"][l])
    P["w_down"] = np.ascontiguousarray(inp["w_down"][l].reshape(D, D))
    P["w_r"] = np.ascontiguousarray(np.concatenate([inp["w_router_group"][l], inp["w_router_expert"][l]], axis=1))
    P["b_r"] = np.ascontiguousarray(np.concatenate([inp["b_router_group"][l], inp["b_router_expert"][l]])[None, :])
    cw = np.concatenate([inp["conv_w"][l], inp["conv_b"][l][None, :]], axis=0)
    P["convp"] = np.ascontiguousarray(cw.reshape(4, 8, 128).transpose(2, 1, 0))
    P["sink"] = np.ascontiguousarray(inp["attn_sink"][l][None, :])
    P["vecs"] = fm(np.stack([inp["mix_norm_g"][l], inp["ln1_g"][l], inp["ln1_b"][l], inp["ln2_g"][l], inp["ln2_b"][l]]))
    P["natb"] = nat_bias_table(np.asarray(inp["nat_rpb"][l]))
    return P


def run_layer(l, h, hc, mod, inp, tabs, cores=range(8)):
    nc, _ = _get("layer", build_layer)
    P = layer_params(l, inp)
    in_maps = [layer_inputs(l, c, h, hc, mod, P, tabs) for c in cores]
    res = run_bass_kernel_spmd(nc, in_maps, core_ids=list(range(len(in_maps))))
    return [r["hout"] for r in res.results]


def kernel(**inp):
    inp = {k: np.asarray(v) for k, v in inp.items()}
    mod = run_ada(inp["c"], inp["c_ctx"], inp["w_ada"], inp["b_ada"])
    tabs = [const_tables(0), const_tables(1)]
    h = np.array(inp["x"], np.float32, copy=True)
    hc = np.array(inp["ctx"], np.float32, copy=True)
    for l in range(NL):
        outs = run_layer(l, h, hc, mod, inp, tabs)
        for c in range(8):
            b, half = c // 2, c % 2
            o = outs[c].reshape(D, OWN).T
            h[b, half * 2048:(half + 1) * 2048] = o[:2048]
            if half == 0:
                hc[b] = o[2048:]
    return h
```
